# Optimizing a Trainium2 kernel written in Bass

```python
import jax, jax.numpy as jnp
from jax import lax
import numpy as np

D_MODEL = 1024
BATCH = 4
SEQ = 4096
DEPTH = 1

HEAD_DIM = 64
SWA_Q_HEADS = 8
SWA_KV_HEADS = 2
SWA_GROUP = SWA_Q_HEADS // SWA_KV_HEADS
WINDOW = 128
FOX_HEADS = 8
Q_BLOCK = 128
N_EXPERTS = 32
TOP_K = 4
D_FF = D_MODEL
SWIGLU_LIMIT = 7.0
SWIGLU_ALPHA = 1.702
EXPERT_BLOCK = 256
LN_EPS = 1e-5
DEEPNORM_ALPHA = (2.0 * DEPTH) ** 0.25
DEEPNORM_BETA = (8.0 * DEPTH) ** -0.25

SWA_Q_W = SWA_Q_HEADS * HEAD_DIM
SWA_KV_W = SWA_KV_HEADS * HEAD_DIM
FOX_W = FOX_HEADS * HEAD_DIM
IN_SPLITS = (SWA_Q_W, SWA_KV_W, SWA_KV_W, FOX_W, FOX_W, FOX_W, FOX_HEADS, D_MODEL, D_MODEL)
IN_WIDTH = sum(IN_SPLITS)

kernel_name = 'hybrid_swa_sink_fox_moe_deepnorm'


def _alibi_slopes(n):
    return jnp.asarray(2.0 ** (-8.0 * np.arange(1, n + 1) / n), dtype=jnp.float32)


def _layer_norm(x, g, b):
    xf = x.astype(jnp.float32)
    mu = jnp.mean(xf, axis=-1, keepdims=True)
    var = jnp.mean(jnp.square(xf - mu), axis=-1, keepdims=True)
    y = (xf - mu) * lax.rsqrt(var + LN_EPS) * g.astype(jnp.float32) + b.astype(jnp.float32)
    return y.astype(x.dtype)


def _sliding_window_sink_attention(q, k, v, sink):
    B, S = q.shape[0], q.shape[1]
    nb = S // WINDOW
    qb = q.reshape(B, nb, WINDOW, SWA_KV_HEADS, SWA_GROUP, HEAD_DIM)

    def with_prev(t):
        tb = t.reshape(B, nb, WINDOW, SWA_KV_HEADS, HEAD_DIM)
        prev = jnp.pad(tb[:, :-1], ((0, 0), (1, 0), (0, 0), (0, 0), (0, 0)))
        return jnp.concatenate([prev, tb], axis=2)

    kx, vx = with_prev(k), with_prev(v)
    scores = jnp.einsum('bnqkgd,bnskd->bnkgqs', qb, kx).astype(jnp.float32) * (HEAD_DIM ** -0.5)
    qi = jnp.arange(WINDOW)[:, None]
    sj = jnp.arange(2 * WINDOW)[None, :]
    dist = qi + WINDOW - sj
    key_pos = jnp.arange(nb)[:, None, None] * WINDOW - WINDOW + sj[None]
    valid = (dist >= 0) & (dist < WINDOW) & (key_pos >= 0)
    slopes = _alibi_slopes(SWA_Q_HEADS).reshape(SWA_KV_HEADS, SWA_GROUP)
    scores = scores - slopes[:, :, None, None] * dist.astype(jnp.float32)
    scores = jnp.where(valid[None, :, None, None], scores, -jnp.inf)
    sink_col = jnp.broadcast_to(sink.astype(jnp.float32)[None, None, :, :, None, None], scores.shape[:-1] + (1,))
    probs = jax.nn.softmax(jnp.concatenate([scores, sink_col], axis=-1), axis=-1)[..., :-1]
    out = jnp.einsum('bnkgqs,bnskd->bnqkgd', probs.astype(v.dtype), vx)
    return out.reshape(B, S, SWA_Q_W)


def _forgetting_attention(q, k, v, log_f):
    B, S = q.shape[0], q.shape[1]
    nb = S // Q_BLOCK
    c = jnp.cumsum(log_f, axis=1)
    c_keys = jnp.transpose(c, (0, 2, 1))
    key_pos = jnp.arange(S)
    qs = jnp.swapaxes(q.reshape(B, nb, Q_BLOCK, FOX_HEADS, HEAD_DIM), 0, 1)
    cs = jnp.swapaxes(c.reshape(B, nb, Q_BLOCK, FOX_HEADS), 0, 1)

    def block(args):
        qb, cb, i = args
        s = jnp.einsum('bqhd,bshd->bhqs', qb, k).astype(jnp.float32) * (HEAD_DIM ** -0.5)
        s = s + jnp.transpose(cb, (0, 2, 1))[..., None] - c_keys[:, :, None, :]
        q_pos = i * Q_BLOCK + jnp.arange(Q_BLOCK)
        s = jnp.where(key_pos[None, :] <= q_pos[:, None], s, -jnp.inf)
        p = jax.nn.softmax(s, axis=-1)
        return jnp.einsum('bhqs,bshd->bqhd', p.astype(v.dtype), v)

    out = lax.map(block, (qs, cs, jnp.arange(nb, dtype=jnp.int32)))
    return jnp.swapaxes(out, 0, 1).reshape(B, S, FOX_W)


def _moe(x, w_router, b_router, w_gate_up, b_gate_up, w_down, b_down):
    B, S, D = x.shape
    N = B * S
    NK = N * TOP_K
    xt = x.reshape(N, D)
    logits = (xt @ w_router + b_router).astype(jnp.float32)
    top_val, top_idx = lax.top_k(logits, TOP_K)
    gates = jax.nn.softmax(top_val, axis=-1)
    flat_e = top_idx.reshape(-1)
    flat_tok = jnp.arange(NK, dtype=jnp.int32) // TOP_K
    flat_w = gates.reshape(-1)
    order = jnp.argsort(flat_e)
    sorted_e = flat_e[order]
    counts = jnp.bincount(flat_e, length=N_EXPERTS)
    padded = ((counts + EXPERT_BLOCK - 1) // EXPERT_BLOCK) * EXPERT_BLOCK
    ends = jnp.cumsum(padded)
    starts = ends - padded
    count_starts = jnp.cumsum(counts) - counts
    rank = jnp.arange(NK, dtype=jnp.int32) - count_starts[sorted_e]
    slot = starts[sorted_e] + rank
    R = ((NK + EXPERT_BLOCK - 1) // EXPERT_BLOCK) * EXPERT_BLOCK + N_EXPERTS * EXPERT_BLOCK
    n_blocks = R // EXPERT_BLOCK
    slot_tok = jnp.zeros((R,), jnp.int32).at[slot].set(flat_tok[order])
    slot_w = jnp.zeros((R,), jnp.float32).at[slot].set(flat_w[order])
    block_e = jnp.minimum(jnp.searchsorted(ends, jnp.arange(n_blocks) * EXPERT_BLOCK, side='right'), N_EXPERTS - 1)
    xs = xt[slot_tok].reshape(n_blocks, EXPERT_BLOCK, D)

    def expert_block(args):
        xb, e = args
        h = xb @ w_gate_up[e] + b_gate_up[e]
        gate, up = h[:, :D_FF], h[:, D_FF:]
        gate = jnp.minimum(gate, SWIGLU_LIMIT)
        up = jnp.clip(up, -SWIGLU_LIMIT, SWIGLU_LIMIT)
        act = gate * jax.nn.sigmoid(gate * SWIGLU_ALPHA) * (up + 1.0)
        return act @ w_down[e] + b_down[e]

    ys = lax.map(expert_block, (xs, block_e)).reshape(R, D)
    out = jnp.zeros((N, D), ys.dtype).at[slot_tok].add(ys * slot_w[:, None].astype(ys.dtype))
    return out.reshape(B, S, D)


def setup_inputs(seed: int = 0) -> dict:
    key = jax.random.key(seed)
    ks = jax.random.split(key, 20)
    beta = DEEPNORM_BETA

    def dense(k, shape, fan_in, scale=1.0):
        return jax.random.normal(k, shape, jnp.float32) * (scale * fan_in ** -0.5)

    kin = jax.random.split(ks[0], len(IN_SPLITS))
    in_scales = (1.0, 1.0, beta, 1.0, 1.0, beta, 1.0, 1.0, 1.0)
    w_in = jnp.concatenate([dense(kin[i], (DEPTH, D_MODEL, w), D_MODEL, in_scales[i]) for i, w in enumerate(IN_SPLITS)], axis=-1)
    return {
        'x': jax.random.normal(ks[1], (BATCH, SEQ, D_MODEL), jnp.float32),
        'w_in': w_in,
        'b_forget': 2.0 + 4.0 * jax.random.uniform(ks[2], (DEPTH, FOX_HEADS), jnp.float32),
        'sink': 0.5 * jax.random.normal(ks[3], (DEPTH, SWA_KV_HEADS, SWA_GROUP), jnp.float32),
        'w_proj_swa': dense(ks[4], (DEPTH, SWA_Q_W, D_MODEL), SWA_Q_W, beta),
        'w_proj_fox': dense(ks[5], (DEPTH, FOX_W, D_MODEL), FOX_W, beta),
        'w_out': dense(ks[6], (DEPTH, D_MODEL, D_MODEL), D_MODEL, beta),
        'ln1_g': 1.0 + 0.02 * jax.random.normal(ks[7], (DEPTH, D_MODEL), jnp.float32),
        'ln1_b': 0.02 * jax.random.normal(ks[8], (DEPTH, D_MODEL), jnp.float32),
        'w_router': dense(ks[9], (DEPTH, D_MODEL, N_EXPERTS), D_MODEL),
        'b_router': 0.01 * jax.random.normal(ks[10], (DEPTH, N_EXPERTS), jnp.float32),
        'w_gate_up': dense(ks[11], (DEPTH, N_EXPERTS, D_MODEL, 2 * D_FF), D_MODEL, beta),
        'b_gate_up': 0.01 * jax.random.normal(ks[12], (DEPTH, N_EXPERTS, 2 * D_FF), jnp.float32),
        'w_down': dense(ks[13], (DEPTH, N_EXPERTS, D_FF, D_MODEL), D_FF, beta),
        'b_down': 0.01 * jax.random.normal(ks[14], (DEPTH, N_EXPERTS, D_MODEL), jnp.float32),
        'ln2_g': 1.0 + 0.02 * jax.random.normal(ks[15], (DEPTH, D_MODEL), jnp.float32),
        'ln2_b': 0.02 * jax.random.normal(ks[16], (DEPTH, D_MODEL), jnp.float32),
    }


def reference(x, w_in, b_forget, sink, w_proj_swa, w_proj_fox, w_out, ln1_g, ln1_b, w_router, b_router, w_gate_up, b_gate_up, w_down, b_down, ln2_g, ln2_b):
    B, S, _ = x.shape
    split_points = [int(p) for p in np.cumsum(IN_SPLITS)[:-1]]
    h = x
    for layer in range(DEPTH):
        proj = h @ w_in[layer]
        qa, ka, va, qf, kf, vf, fl, ga, gf = jnp.split(proj, split_points, axis=-1)
        ya = _sliding_window_sink_attention(
            qa.reshape(B, S, SWA_KV_HEADS, SWA_GROUP, HEAD_DIM),
            ka.reshape(B, S, SWA_KV_HEADS, HEAD_DIM),
            va.reshape(B, S, SWA_KV_HEADS, HEAD_DIM),
            sink[layer]) @ w_proj_swa[layer]
        log_f = jax.nn.log_sigmoid(fl.astype(jnp.float32) + b_forget[layer].astype(jnp.float32))
        yf = _forgetting_attention(
            qf.reshape(B, S, FOX_HEADS, HEAD_DIM),
            kf.reshape(B, S, FOX_HEADS, HEAD_DIM),
            vf.reshape(B, S, FOX_HEADS, HEAD_DIM),
            log_f) @ w_proj_fox[layer]
        mix = jax.nn.sigmoid(ga) * ya + jax.nn.sigmoid(gf) * yf
        h = _layer_norm(DEEPNORM_ALPHA * h + mix @ w_out[layer], ln1_g[layer], ln1_b[layer])
        m = _moe(h, w_router[layer], b_router[layer], w_gate_up[layer], b_gate_up[layer], w_down[layer], b_down[layer])
        h = _layer_norm(DEEPNORM_ALPHA * h + m, ln2_g[layer], ln2_b[layer])
    return h
```

```python
import contextlib
import numpy as np
import concourse.bass as bass
import concourse.mybir as mybir
from concourse.bass_utils import run_bass_kernel_spmd

F32 = mybir.dt.float32
BF16 = mybir.dt.bfloat16
AF = mybir.ActivationFunctionType
ALU = mybir.AluOpType
AX = mybir.AxisListType

NB = 32
NOWN = 16
NE = 32
ALPHA = float(2.0 ** 0.25)
EPS = 1e-5
NEG = -30000.0
CAP = 384
NSLOT = NE * CAP
N_PRE = 14
E_PRE0 = NE - N_PRE
C_SWAQ, C_SWAK, C_SWAV, C_FOXQ, C_FOXK, C_FOXV, C_FL, C_GA, C_GF = 0, 512, 640, 768, 1280, 1792, 2304, 2312, 3336


class Res:
    __slots__ = ("name", "last_w", "readers")

    def __init__(self, name=""):
        self.name = name
        self.last_w = None
        self.readers = {}


class Sched:
    NDMA = 96
    NSP = 32

    def __init__(self, nc, same_engine_wait=True):
        self.nc = nc
        self.engs = ["pe", "act", "dve", "pool", "sp"]
        self.prog = {k: [] for k in self.engs}
        self.cnt = {k: 0 for k in self.engs}
        self.waited = {k: {} for k in self.engs}
        self.dma_i = {"sp": 0, "pool": 0, "act": 0}
        self.dma_cnt = [0] * self.NDMA
        self.same_engine_wait = same_engine_wait

    def _deps(self, reads, writes):
        deps = {}

        def add(k, v):
            if deps.get(k, 0) < v:
                deps[k] = v
        for r in reads:
            if r.last_w is not None:
                add(*r.last_w)
        for w in writes:
            if w.last_w is not None:
                add(*w.last_w)
            for k, v in w.readers.items():
                add(k, v)
        return deps

    def _filter(self, eng, deps):
        out = []
        wd = self.waited[eng]
        for k, v in deps.items():
            if k == eng and (not self.same_engine_wait or eng in ("pe", "sp")):
                continue
            if wd.get(k, 0) >= v:
                continue
            wd[k] = v
            out.append((k, v))
        return out

    def _mark(self, tok, reads, writes):
        for w in writes:
            w.last_w = tok
            w.readers = {}
        k, v = tok
        for r in reads:
            if r in writes:
                continue
            if r.readers.get(k, 0) < v:
                r.readers[k] = v

    def op(self, eng, name, reads=(), writes=(), inc=True, **kw):
        waits = self._filter(eng, self._deps(reads, writes))
        if inc:
            self.cnt[eng] += 1
            tok = (eng, self.cnt[eng])
        else:
            tok = (eng, self.cnt[eng] + 1)
        self.prog[eng].append((waits, name, kw, eng if inc else None))
        self._mark(tok, reads, writes)
        return tok

    def dma(self, qeng, reads=(), writes=(), name="dma_start", **kw):
        deps = self._deps(reads, writes)
        if qeng == "sp":
            i = self.dma_i[qeng]
            self.dma_i[qeng] = (self.dma_i[qeng] + 1) % self.NSP
        else:
            i = self.NSP + self.dma_i[qeng]
            self.dma_i[qeng] = (self.dma_i[qeng] + 1) % (self.NDMA - self.NSP)
        if self.dma_cnt[i] > 0:
            deps[("d", i)] = max(deps.get(("d", i), 0), self.dma_cnt[i])
        waits = self._filter(qeng, deps)
        self.dma_cnt[i] += 16
        tok = (("d", i), self.dma_cnt[i])
        self.prog[qeng].append((waits, name, kw, ("d", i)))
        self._mark(tok, reads, writes)
        return tok

    def barrier(self):
        deps = {k: self.cnt[k] for k in self.engs if self.cnt[k] > 0}
        for i in range(self.NDMA):
            if self.dma_cnt[i] > 0:
                deps[("d", i)] = self.dma_cnt[i]
        for eng in self.engs:
            d = {k: v for k, v in deps.items() if k != eng}
            waits = self._filter(eng, d)
            self.prog[eng].append((waits, None, None, None))

    def emit(self):
        nc = self.nc
        with contextlib.ExitStack() as st:
            sems = {}
            for k in self.engs:
                sems[k] = st.enter_context(nc.semaphore("s_" + k))
            for i in range(self.NDMA):
                sems[("d", i)] = st.enter_context(nc.semaphore("s_d%d" % i))
            block = st.enter_context(nc.Block())

            def run(name, e):
                for waits, op, kw, inc in self.prog[name]:
                    for k, v in waits:
                        e.wait_ge(sems[k], v)
                    if op is None:
                        continue
                    ins = getattr(e, op)(**kw)
                    if inc is None:
                        continue
                    ins.then_inc(sems[inc], 16 if isinstance(inc, tuple) else 1)

            block.tensor(lambda e: run("pe", e))
            block.scalar(lambda e: run("act", e))
            block.vector(lambda e: run("dve", e))
            block.gpsimd(lambda e: run("pool", e))
            block.sync(lambda e: run("sp", e))


class Arena:
    def __init__(self, nc, base=16512, limit=16512 + 207 * 1024):
        self.nc = nc
        self.off = base
        self.limit = limit
        self.top = limit
        self.n = 0

    def alloc_top(self, shape, dtype):
        esz = 2 if dtype == BF16 else 4
        per = esz * int(np.prod(shape[1:]))
        per = (per + 63) // 64 * 64
        self.top -= per
        assert self.off <= self.top, ("SBUF overflow", self.off, self.top)
        self.n += 1
        return self.nc.alloc_sbuf_tensor_at("t%d" % self.n, list(shape), dtype, offset=self.top)

    def alloc(self, shape, dtype):
        esz = 2 if dtype == BF16 else 4
        per = esz * int(np.prod(shape[1:]))
        per = (per + 63) // 64 * 64
        off = self.off
        self.off += per
        assert self.off <= self.top, ("SBUF overflow", self.off, self.top)
        self.n += 1
        return self.nc.alloc_sbuf_tensor_at("t%d" % self.n, list(shape), dtype, offset=off)

    def mark(self):
        return self.off

    def release(self, m):
        self.off = m


def bc_last(ap, n):
    return bass.AP(ap.tensor, ap.offset, [list(x) for x in ap.ap] + [[0, n]])


def midbc(ap2, n, step=0):
    return bass.AP(ap2.tensor, ap2.offset, [list(ap2.ap[0]), [step, n], list(ap2.ap[1])])


def build_nc(debug=False):
    nc = bass.Bass("TRN2", target_bir_lowering=False)

    def din(name, shape):
        return nc.dram_tensor(name, list(shape), F32, kind="ExternalInput").ap()
    xT3 = din("xT3", [128, 8, 4096])
    xTw3 = din("xTw3", [128, 8, 4096])
    xTo3 = din("xTo3", [128, 8, 2048])
    xo = din("xo", [2048, 1024])
    wfm = din("wfm", [29, 128, 1024])
    wtm = din("wtm", [128, 8, 648])
    wpa = din("wpa", [128, 4, 1024])
    wpf = din("wpf", [128, 4, 1024])
    wout = din("wout", [128, 8, 1024])
    ln1g = din("ln1g", [128, 1024])
    ln1b = din("ln1b", [128, 1024])
    ln2g = din("ln2g", [128, 1024])
    ln2b = din("ln2b", [128, 1024])
    wr = din("wr", [128, 8, 32])
    br = din("br", [128, 32])
    bfr = din("bfr", [128, 256])
    sink8 = din("sink8", [128, 8])
    swatbl = din("swatbl", [128, 3 * 2 * 4 * 128])
    maskpair = din("maskpair", [128, 16 * 2 * 128])
    par = din("par", [128, 16])
    ident_d = din("ident", [128, 128])
    triu_d = din("triu", [128, 128])
    wgu = din("wgu", [NE, 16, 128, 1024])
    wd = din("wd", [NE, 8, 128, 1024])
    bgu = din("bgu", [128, NE * 16])
    bd = din("bd", [NE, 1024])
    ebase = din("ebase", [128, 32])
    striu_d = din("striu", [128, 128])
    xs_d = nc.dram_tensor("xs_scratch", [NSLOT + 128, 1024], BF16, kind="Internal").ap()
    ys_d = nc.dram_tensor("ys_scratch", [NSLOT + 128, 1024], F32, kind="Internal").ap()
    r_xs = Res("xs")
    wgu_b = nc.dram_tensor("wgu_bf16", [N_PRE, 16, 128, 1024], BF16, kind="Internal").ap()
    wd_b = nc.dram_tensor("wd_bf16", [N_PRE, 8, 128, 1024], BF16, kind="Internal").ap()
    out = nc.dram_tensor("out", [2048, 1024], F32, kind="ExternalOutput").ap()
    if debug:
        dbg_h1 = nc.dram_tensor("dbg_h1", [2048, 1024], F32, kind="ExternalOutput").ap()
        dbg_G = nc.dram_tensor("dbg_G", [2048, 32], F32, kind="ExternalOutput").ap()
        dbg_swa = nc.dram_tensor("dbg_swa", [2048, 512], F32, kind="ExternalOutput").ap()
        dbg_fox = nc.dram_tensor("dbg_fox", [2048, 512], F32, kind="ExternalOutput").ap()

    S = Sched(nc)
    A = Arena(nc)

    PS = [nc.alloc_psum_tensor("ps%d" % i, [128, 512], F32) for i in range(7)]
    PSR = [Res("ps%d" % i) for i in range(7)]
    PST = nc.alloc_psum_tensor("pst", [128, 1024], BF16)
    PSTR = Res("pst")

    def mm(out_, lhsT, rhs, start, stop, reads, writes, last=None):
        S.op("pe", "matmul", reads=reads, writes=writes, inc=(stop if last is None else last),
             out=out_, lhsT=lhsT, rhs=rhs, start=start, stop=stop)

    def act(out_, in_, func, reads, writes, **kw):
        S.op("act", "activation", reads=reads, writes=writes, out=out_, in_=in_, func=func, **kw)

    ident_f = A.alloc([128, 128], F32)
    ident_b = A.alloc([128, 128], BF16)
    ones_f = A.alloc([128, 128], F32)
    r_const = Res("const")
    S.dma("sp", writes=[r_const], out=ident_f[:], in_=ident_d)
    S.dma("pool", writes=[r_const], out=ident_b[:], in_=ident_d)
    S.op("dve", "memset", writes=[r_const], ap=ones_f[:], constant=1.0)

    swa_out = A.alloc_top([128, NOWN, 512], BF16)
    fox_out = A.alloc_top([128, NOWN, 512], BF16)
    r_swa_out = [Res() for _ in range(NOWN)]
    r_fox_out = [Res() for _ in range(NOWN)]
    m_attn = A.mark()

    KTf = A.alloc([128, 4, 4096], BF16)
    QTf = A.alloc([128, 4, 2048], BF16)
    Vf = A.alloc([128, NB, 8, 65], BF16)
    KTs = A.alloc([128, 4096], BF16)
    QTs = A.alloc([128, 4, 2048], BF16)
    Vs = A.alloc([128, NB, 2, 65], BF16)
    r_KTf = [[Res() for _ in range(8)] for _ in range(4)]
    r_QTf = [[Res() for _ in range(4)] for _ in range(4)]
    r_Vf = [Res() for _ in range(NB)]
    r_KTs = [Res() for _ in range(8)]
    r_QTs = [[Res() for _ in range(4)] for _ in range(4)]
    r_Vs = [Res() for _ in range(NB)]
    zt = A.alloc([128, 256], F32)
    lt = A.alloc([128, 256], F32)
    cw = A.alloc([128, 256], F32)
    Tb = A.alloc([128, 256], F32)
    cend = A.alloc([128, 256], F32)
    cpos = A.alloc([128, 256], F32)
    cendo = A.alloc([128, NOWN, 8], F32)
    kscA = A.alloc([128, NB, 8], F32)
    kscB = A.alloc([128, 8, 2, 8], F32)
    ub = A.alloc([128, NOWN, 8, 8], F32)
    ubl = A.alloc([128, NOWN, 8], F32)
    bfr_sb = A.alloc([128, 256], F32)
    par_sb = A.alloc([128, 16], F32)
    triu_f = A.alloc([128, 128], F32)
    esink = A.alloc([128, 8], F32)
    r_fl = Res("fl")
    m_p12 = A.mark()

    xg = [A.alloc([128, 8, 512], BF16) for _ in range(2)]
    r_xg = [Res() for _ in range(2)]
    wres = A.alloc([128, 8, 1024], BF16)
    r_wres = Res("wres")
    wtm_sb = A.alloc([128, 8, 648], BF16)
    r_wtm = Res("wtm")

    S.dma("pool", writes=[r_wres], out=wres[:, 0:5, :], in_=wfm[0:5].rearrange("n p f -> p n f"))
    S.dma("pool", writes=[r_wtm], out=wtm_sb[:], in_=wtm)
    S.dma("sp", writes=[r_fl], out=bfr_sb[:], in_=bfr)
    S.dma("sp", writes=[r_fl], out=par_sb[:], in_=par)
    S.dma("sp", writes=[r_fl], out=triu_f[:], in_=triu_d)
    S.dma("sp", writes=[r_fl], out=esink[:], in_=sink8)
    S.op("dve", "memset", writes=r_Vf, ap=Vf[:, :, :, 64:65], constant=1.0)
    S.op("dve", "memset", writes=r_Vs, ap=Vs[:, :, :, 64:65], constant=1.0)

    def wpiece(n):
        return wres[:, (n if n < 5 else n - 5), :].rearrange("p (c f) -> p c f", c=8)

    bank_i = [0]

    def nbank(lo=0, hi=6):
        b = lo + bank_i[0] % (hi - lo)
        bank_i[0] += 1
        return b

    FLB = 6
    xg_i = [0]

    def load_xg(src):
        s = xg_i[0] % 2
        xg_i[0] += 1
        S.dma("pool", writes=[r_xg[s]], out=xg[s][:], in_=src)
        return s

    def proj_fm(s, piece, dst, r_dst, scale):
        b = nbank()
        wp = wpiece(piece)
        for c in range(8):
            mm(PS[b][:, :], wp[:, c, :], xg[s][:, c, :], c == 0, c == 7,
               reads=[r_wres, r_xg[s]], writes=[PSR[b]])
        act(dst, PS[b][:, :], AF.Copy, reads=[PSR[b]], writes=[r_dst], scale=scale)

    for g in range(8):
        s = load_xg(xT3[:, :, g * 512:(g + 1) * 512])
        for j in range(4):
            proj_fm(s, j, KTf[:, j, g * 512:(g + 1) * 512], r_KTf[j][g], 1.0)
        for blk in range(4):
            J = 4 * g + blk
            b = nbank()
            for c in range(8):
                mm(PS[b][:, :], xg[s][:, c, blk * 128:(blk + 1) * 128], wtm_sb[:, c, 0:512], c == 0, c == 7,
                   reads=[r_wtm, r_xg[s]], writes=[PSR[b]])
            S.op("dve", "tensor_copy", reads=[PSR[b]], writes=[r_Vf[J]],
                 out=Vf[:, J, :, 0:64], in_=PS[b][:, :].rearrange("p (h d) -> p h d", h=8))
            for c in range(8):
                mm(PS[FLB][:, J * 8:(J + 1) * 8], xg[s][:, c, blk * 128:(blk + 1) * 128], wtm_sb[:, c, 512:520],
                   c == 0, c == 7, reads=[r_wtm, r_xg[s]], writes=[PSR[FLB]])
    S.op("dve", "tensor_tensor", reads=[PSR[FLB], r_fl], writes=[r_fl], out=zt[:], in0=PS[FLB][:, 0:256],
         in1=bfr_sb[:], op=ALU.add)
    act(lt[:], zt[:], AF.Exp, reads=[r_fl], writes=[r_fl], scale=-1.0)
    act(lt[:], lt[:], AF.Ln, reads=[r_fl], writes=[r_fl], bias=1.0)
    act(esink[:], esink[:], AF.Exp, reads=[r_fl], writes=[r_fl])
    mm(PS[0][:, 0:256], triu_f[:], lt[:], True, True, reads=[r_fl], writes=[PSR[0]])
    mm(PS[1][:, 0:256], ones_f[:], lt[:], True, True, reads=[r_fl, r_const], writes=[PSR[1]])
    S.op("dve", "tensor_copy", reads=[PSR[0]], writes=[r_fl], out=cw[:], in_=PS[0][:, 0:256])
    S.op("dve", "tensor_copy", reads=[PSR[1]], writes=[r_fl], out=Tb[:], in_=PS[1][:, 0:256])
    Tb3 = Tb[:].rearrange("p (j h) -> p j h", h=8)
    cend3 = cend[:].rearrange("p (j h) -> p j h", h=8)
    cpos3 = cpos[:].rearrange("p (j h) -> p j h", h=8)
    for h in range(8):
        S.op("dve", "tensor_tensor_scan", reads=[r_fl, r_const], writes=[r_fl], out=cend3[:, :, h],
             data0=ones_f[:, 0:32], data1=Tb3[:, :, h], initial=0.0, op0=ALU.mult, op1=ALU.add)
    S.op("dve", "tensor_tensor", reads=[r_fl], writes=[r_fl], out=cpos[:], in0=cend[:], in1=Tb[:], op=ALU.subtract)
    S.op("dve", "tensor_tensor", reads=[r_fl], writes=[r_fl], out=cpos[:], in0=cpos[:], in1=cw[:], op=ALU.add)
    for i in range(NOWN):
        S.op("dve", "scalar_tensor_tensor", reads=[r_fl], writes=[r_fl], out=cendo[:, i, :], in0=Tb3[:, 2 * i + 1, :],
             scalar=par_sb[:, i:i + 1], in1=cend3[:, 2 * i, :], op0=ALU.mult, op1=ALU.add)

    for m_ in range(8):
        S.op("dve", "tensor_tensor", reads=[r_fl], writes=[r_fl], out=kscA[:, 4 * m_:4 * m_ + 4, :],
             in0=cpos3[:, 4 * m_:4 * m_ + 4, :], in1=midbc(cend3[:, 4 * m_ + 3, :], 4), op=ALU.subtract)
    act(kscA[:], kscA[:], AF.Exp, reads=[r_fl], writes=[r_fl])
    for ie in range(8):
        i_ = 2 * ie
        S.op("dve", "tensor_tensor", reads=[r_fl], writes=[r_fl], out=kscB[:, ie, :, :],
             in0=cpos3[:, 2 * i_:2 * i_ + 2, :], in1=midbc(cend3[:, 2 * i_ + 1, :], 2), op=ALU.subtract)
    act(kscB[:], kscB[:], AF.Exp, reads=[r_fl], writes=[r_fl])
    for i_ in range(NOWN):
        nfull = (2 * i_ + 2) // 4
        if nfull > 0:
            S.op("dve", "tensor_tensor", reads=[r_fl], writes=[r_fl], out=ub[:, i_, 0:nfull, :],
                 in0=midbc(cend3[:, 3, :], nfull, step=32), in1=midbc(cendo[:, i_, :], nfull), op=ALU.subtract)
    S.op("dve", "tensor_tensor", reads=[r_fl], writes=[r_fl], out=ubl[:], in0=midbc(cend3[:, 1, :], NOWN, step=16),
         in1=cendo[:], op=ALU.subtract)

    for g in range(8):
        s = load_xg(xTw3[:, :, g * 512:(g + 1) * 512])
        proj_fm(s, 4, KTs[:, g * 512:(g + 1) * 512], r_KTs[g], 1.0)
        for blk in range(4):
            Jw = 4 * g + blk
            b = nbank()
            for c in range(8):
                mm(PS[b][:, 0:128], xg[s][:, c, blk * 128:(blk + 1) * 128], wtm_sb[:, c, 520:648], c == 0, c == 7,
                   reads=[r_wtm, r_xg[s]], writes=[PSR[b]])
            S.op("dve", "tensor_copy", reads=[PSR[b]], writes=[r_Vs[Jw]],
                 out=Vs[:, Jw, :, 0:64], in_=PS[b][:, 0:128].rearrange("p (h d) -> p h d", h=2))
    S.dma("pool", reads=[], writes=[r_wres], out=wres[:, 0:4, :], in_=wfm[5:9].rearrange("n p f -> p n f"))
    S.dma("pool", reads=[], writes=[r_wres], out=wres[:, 4:8, :], in_=wfm[9:13].rearrange("n p f -> p n f"))
    for og in range(4):
        s = load_xg(xTo3[:, :, og * 512:(og + 1) * 512])
        for j in range(4):
            proj_fm(s, 5 + j, QTf[:, j, og * 512:(og + 1) * 512], r_QTf[j][og], 0.125)
        for r in range(4):
            proj_fm(s, 9 + r, QTs[:, r, og * 512:(og + 1) * 512], r_QTs[r][og], 0.125)

    S.barrier()
    A.release(m_p12)
    tbl = A.alloc([128, 3, 2, 512], F32)
    mpair = A.alloc([128, 16, 2, 128], BF16)
    PT = [A.alloc([128, 512], BF16) for _ in range(5)]
    r_PT = [Res() for _ in range(5)]
    r_PTs = [[Res() for _ in range(4)] for _ in range(5)]
    Stmp = [A.alloc([128, 512], F32) for _ in range(4)]
    r_Stmp = [Res() for _ in range(4)]
    rden = A.alloc([128, 2, 8], F32)
    r_rden = [Res(), Res()]
    r_tbl = Res("tbl")
    r_bias = Res("bias")
    S.dma("sp", writes=[r_tbl], out=tbl[:].rearrange("p a b c -> p (a b c)"), in_=swatbl)
    S.dma("pool", writes=[r_tbl], out=mpair[:].rearrange("p a b c -> p (a b c)"), in_=maskpair)
    zt_b = A.alloc([128, 2048], BF16)
    r_zt = Res("zt")
    S.op("pool", "memset", writes=[r_zt], ap=zt_b[:], constant=0.0)
    xs_flat = xs_d[0:NSLOT, :].rearrange("(n p r) f -> n p (r f)", p=128, r=2)
    for n_ in range(NSLOT // 256):
        S.dma("sp", reads=[r_zt], writes=[], out=xs_flat[n_], in_=zt_b[:])
    S.dma("pool", reads=[r_zt], writes=[], out=ys_d[NSLOT:NSLOT + 128, :], in_=zt_b[:, 0:1024])
    for pe_ in range(N_PRE):
        for j_ in range(16):
            S.dma("pool", out=wgu_b[pe_, j_], in_=wgu[E_PRE0 + pe_, j_])
        for k_ in range(8):
            S.dma("pool", out=wd_b[pe_, k_], in_=wd[E_PRE0 + pe_, k_])
    OB = [4, 5, 6]
    fin_i = [0]
    swa_it = [(i, g) for i in range(NOWN) for g in range(2)]

    def swa_S(n):
        i, g = swa_it[n]
        og = i // 4
        for w in range(2):
            b = (2 * n + w) % 4
            Jw = 2 * i + w
            mm(PS[b][:, :].rearrange("p (r q) -> p r q", r=4), KTs[g * 64:(g + 1) * 64, Jw * 128:(Jw + 1) * 128],
               QTs[g * 64:(g + 1) * 64, :, i * 128:(i + 1) * 128], True, True,
               reads=[r_KTs[Jw // 4]] + [r_QTs[r][og] for r in range(4)], writes=[PSR[b]])
            t = 0 if w == 1 else (2 if i == 0 else 1)
            S.op("dve", "tensor_tensor", reads=[PSR[b], r_tbl], writes=[r_Stmp[b]], out=Stmp[b][:],
                 in0=PS[b][:, :], in1=tbl[:, t, g, :], op=ALU.add)
            act(PT[b][:], Stmp[b][:], AF.Exp, reads=[r_Stmp[b]], writes=[r_PT[b]])

    def swa_PV(n):
        i, g = swa_it[n]
        ob = OB[n % 3]
        O3 = PS[ob][:, :].rearrange("p (r q) -> p r q", r=4)
        for r in range(4):
            for w in range(2):
                Jw = 2 * i + w
                pb = (2 * n + w) % 4
                mm(O3[:, r, 0:65], PT[pb][:, r * 128:(r + 1) * 128], Vs[:, Jw, g, :], w == 0, w == 1,
                   reads=[r_PT[pb], r_Vs[Jw]], writes=[PSR[ob]], last=(w == 1 and r == 3))
        k = fin_i[0] % 2
        fin_i[0] += 1
        S.op("dve", "tensor_tensor", reads=[PSR[ob], r_fl], writes=[r_rden[k]], out=rden[:, k, 0:4],
             in0=O3[:, :, 64], in1=esink[:, 4 * g:4 * g + 4], op=ALU.add)
        S.op("dve", "reciprocal", reads=[r_rden[k]], writes=[r_rden[k]], out=rden[:, k, 0:4], in_=rden[:, k, 0:4])
        for r in range(4):
            h = 4 * g + r
            S.op("dve", "tensor_scalar", reads=[PSR[ob], r_rden[k]], writes=[r_swa_out[i]],
                 out=swa_out[:, i, h * 64:(h + 1) * 64], in0=O3[:, r, 0:64], scalar1=rden[:, k, r:r + 1],
                 scalar2=None, op0=ALU.mult)

    swa_S(0)
    for n in range(len(swa_it)):
        if n + 1 < len(swa_it):
            swa_S(n + 1)
        swa_PV(n)

    chunks = []
    for i in range(NOWN):
        nj = 2 * i + 2
        for h in range(8):
            for j0 in range(0, nj, 4):
                chunks.append((i, h, list(range(j0, min(j0 + 4, nj)))))

    def fox_S(n):
        i, h, Js = chunks[n]
        b = n % 5
        j, hp = h // 2, h % 2
        for s_, J in enumerate(Js):
            mm(PS[b][:, s_ * 128:(s_ + 1) * 128], KTf[hp * 64:(hp + 1) * 64, j, J * 128:(J + 1) * 128],
               QTf[hp * 64:(hp + 1) * 64, j, i * 128:(i + 1) * 128], True, True,
               reads=[r_KTf[j][J // 4], r_QTf[j][i // 4]], writes=[PSR[b]], last=(s_ == len(Js) - 1))

    def fox_PV(n):
        i, h, Js = chunks[n]
        b = n % 5
        nj = 2 * i + 2
        ob = 5 + h // 4
        O3 = PS[ob][:, :].rearrange("p (r q) -> p r q", r=4)
        L = len(Js)
        m_ = Js[0] // 4
        partial = (Js[-1] != 4 * m_ + 3)
        assert (not partial) or (i % 2 == 0 and L == 2)
        bias_ap = ubl[:, i, h:h + 1] if partial else ub[:, i, m_, h:h + 1]
        scl_ap = kscB[:, i // 2, :, h] if partial else kscA[:, 4 * m_:4 * m_ + 4, h]
        pt_res = [r_PT[b]] + r_PTs[b][0:L]
        act(PT[b][:, 0:L * 128], PS[b][:, 0:L * 128], AF.Exp, reads=[PSR[b], r_fl], writes=pt_res, bias=bias_ap)
        S.op("dve", "tensor_tensor", reads=[r_fl] + pt_res, writes=pt_res,
             out=PT[b][:, 0:L * 128].rearrange("p (s q) -> p s q", s=L),
             in0=PT[b][:, 0:L * 128].rearrange("p (s q) -> p s q", s=L), in1=bc_last(scl_ap, 128), op=ALU.mult)
        for s_, J in enumerate(Js):
            if J >= 2 * i:
                S.op("dve", "tensor_tensor", reads=[r_PTs[b][s_], r_tbl], writes=[r_PTs[b][s_]],
                     out=PT[b][:, s_ * 128:(s_ + 1) * 128], in0=PT[b][:, s_ * 128:(s_ + 1) * 128],
                     in1=mpair[:, i, J - 2 * i, :], op=ALU.mult)
        for s_, J in enumerate(Js):
            mm(O3[:, h % 4, 0:65], PT[b][:, s_ * 128:(s_ + 1) * 128], Vf[:, J, h, :], J == 0, J == nj - 1,
               reads=[r_PT[b], r_PTs[b][s_], r_Vf[J]], writes=[PSR[ob]], last=(s_ == len(Js) - 1))
        if Js[-1] == nj - 1 and h % 4 == 3:
            k = fin_i[0] % 2
            fin_i[0] += 1
            S.op("dve", "reciprocal", reads=[PSR[ob]], writes=[r_rden[k]], out=rden[:, k, 0:4], in_=O3[:, :, 64])
            for r in range(4):
                hh = (h // 4) * 4 + r
                S.op("dve", "tensor_scalar", reads=[PSR[ob], r_rden[k]], writes=[r_fox_out[i]],
                     out=fox_out[:, i, hh * 64:(hh + 1) * 64], in0=O3[:, r, 0:64], scalar1=rden[:, k, r:r + 1],
                     scalar2=None, op0=ALU.mult)

    DEPTH = 3
    for n in range(min(DEPTH, len(chunks))):
        fox_S(n)
    for n in range(len(chunks)):
        if n + DEPTH < len(chunks):
            fox_S(n + DEPTH)
        fox_PV(n)

    if debug:
        S.dma("pool", reads=r_swa_out, out=dbg_swa.rearrange("(i p) f -> p i f", p=128), in_=swa_out[:])
        S.dma("pool", reads=r_fox_out, out=dbg_fox.rearrange("(i p) f -> p i f", p=128), in_=fox_out[:])
    S.barrier()
    A.release(m_attn)
    h1 = A.alloc([128, NOWN, 1024], F32)
    r_h1 = [Res() for _ in range(NOWN)]
    G = A.alloc([128, NOWN, 32], F32)
    gk = A.alloc([128, NOWN, 4], F32)
    slots_i = A.alloc([128, NOWN, 4], mybir.dt.int32)
    r_route = [Res() for _ in range(NOWN)]
    m_h1 = A.mark()
    lgall = A.alloc([128, NOWN, 32], F32)
    r_lg = [Res() for _ in range(NOWN)]
    m_r = A.mark()
    wr_sb = A.alloc([128, 8, 32], F32)
    br_sb = A.alloc([128, 32], F32)
    r_w4r = Res("w4r")
    S.dma("sp", writes=[r_w4r], out=wr_sb[:], in_=wr)
    S.dma("sp", writes=[r_w4r], out=br_sb[:], in_=br)
    hTf = A.alloc([128, 8, 128], F32)
    r_hTf = Res()
    wpa_sb = A.alloc([128, 4, 1024], BF16)
    wpf_sb = A.alloc([128, 4, 1024], BF16)
    wout_sb = A.alloc([128, 8, 1024], BF16)
    r_w3 = Res("w3")
    S.dma("pool", writes=[r_w3], out=wpa_sb[:], in_=wpa)
    S.dma("pool", writes=[r_w3], out=wpf_sb[:], in_=wpf)
    S.dma("pool", writes=[r_w3], out=wout_sb[:], in_=wout)
    g1 = A.alloc([128, 1024], F32)
    b1 = A.alloc([128, 1024], F32)
    S.dma("sp", writes=[r_w3], out=g1[:], in_=ln1g)
    S.dma("sp", writes=[r_w3], out=b1[:], in_=ln1b)
    xq = [A.alloc([128, 8, 512], BF16) for _ in range(1)]
    r_xq = [Res()]
    NWR = 6
    wring = [A.alloc([128, 8, 128], BF16) for _ in range(NWR)]
    r_wring = [Res() for _ in range(NWR)]
    aT = A.alloc([128, 4, 512], BF16)
    fT = A.alloc([128, 4, 512], BF16)
    r_aT, r_fT = Res(), Res()
    mixT = [A.alloc([128, 8, 512], BF16) for _ in range(2)]
    r_mixT = [[Res() for _ in range(8)] for _ in range(2)]
    sg = [A.alloc([128, 512], F32) for _ in range(4)]
    r_sg = [Res() for _ in range(4)]
    xres = [A.alloc([128, 1024], F32) for _ in range(1)]
    r_xres = [Res()]
    lnt = A.alloc([128, 1024], BF16)
    r_lnt = Res()
    stat = A.alloc([128, 4, 8], F32)
    r_stat = [Res() for _ in range(4)]

    ln_i = [0]

    def ln_stages(src, r_src, dst, r_dst, gam, bet, r_gb):
        k_ = ln_i[0] % 4
        ln_i[0] += 1
        st_ = stat[:, k_, :]
        rs = r_stat[k_]

        def s0():
            S.op("dve", "memset", writes=[rs], ap=st_[:, 0:4], constant=0.0)
            act(lnt[:], src, AF.Identity, reads=[r_src, rs], writes=[rs], accum_out=st_[:, 0:1])

        def s1():
            S.op("dve", "tensor_scalar", reads=[rs], writes=[rs], out=st_[:, 1:2], in0=st_[:, 0:1],
                 scalar1=-1.0 / 1024, scalar2=None, op0=ALU.mult)
            act(lnt[:], src, AF.Square, reads=[r_src, rs], writes=[rs], bias=st_[:, 1:2], accum_out=st_[:, 2:3])

        def s2():
            S.op("dve", "tensor_scalar", reads=[rs], writes=[rs], out=st_[:, 3:4], in0=st_[:, 2:3],
                 scalar1=1.0 / 1024, scalar2=EPS, op0=ALU.mult, op1=ALU.add)
            act(st_[:, 5:6], st_[:, 3:4], AF.Sqrt, reads=[rs], writes=[rs])

        def s3():
            S.op("dve", "reciprocal", reads=[rs], writes=[rs], out=st_[:, 4:5], in_=st_[:, 5:6])
            S.op("dve", "tensor_tensor", reads=[rs], writes=[rs], out=st_[:, 6:7], in0=st_[:, 1:2], in1=st_[:, 4:5],
                 op=ALU.mult)
            act(dst, src, AF.Identity, reads=[r_src, rs], writes=[r_dst], scale=st_[:, 4:5], bias=st_[:, 6:7])

        def s4():
            S.op("dve", "tensor_tensor", reads=[r_dst, r_gb], writes=[r_dst], out=dst, in0=dst, in1=gam, op=ALU.mult)
            S.op("dve", "tensor_tensor", reads=[r_dst, r_gb], writes=[r_dst], out=dst, in0=dst, in1=bet, op=ALU.add)
        return [s0, s1, s2, s3, s4]

    def layer_norm(src, r_src, dst, r_dst, gam, bet, r_gb):
        for f_ in ln_stages(src, r_src, dst, r_dst, gam, bet, r_gb):
            f_()

    def route_pe(i):
        for half in range(2):
            b = nbank(0, 7)
            for cc in range(4):
                c = half * 4 + cc
                S.op("pe", "transpose", reads=[r_h1[i], r_const], writes=[PSR[b]], inc=(cc == 3),
                     out=PS[b][:, cc * 128:(cc + 1) * 128], in_=h1[:, i, c * 128:(c + 1) * 128], identity=ident_f[:])
            S.op("dve", "tensor_copy", reads=[PSR[b]], writes=[r_hTf],
                 out=hTf[:, half * 4:(half + 1) * 4, :], in_=PS[b][:, :].rearrange("p (c t) -> p c t", c=4))
        b = nbank(0, 7)
        for c in range(8):
            mm(PS[b][:, 0:32], hTf[:, c, :], wr_sb[:, c, :], c == 0, c == 7, reads=[r_hTf, r_w4r], writes=[PSR[b]])
        S.op("dve", "tensor_tensor", reads=[PSR[b], r_w4r], writes=[r_lg[i]], out=lgall[:, i, :], in0=PS[b][:, 0:32],
             in1=br_sb[:], op=ALU.add)

    wr_i = [0]

    def gate_prep(og):
        S.dma("pool", writes=[r_xq[0]], out=xq[0][:], in_=xTo3[:, :, og * 512:(og + 1) * 512])
        for src, r_src, dstT, r_dstT in ((swa_out, r_swa_out, aT, r_aT), (fox_out, r_fox_out, fT, r_fT)):
            for half in range(2):
                for bb in range(2):
                    i = og * 4 + half * 2 + bb
                    for r in range(4):
                        S.op("pe", "transpose", reads=[r_src[i], r_const], writes=[PSTR],
                             inc=(bb == 1 and r == 3),
                             out=PST[:, (bb * 4 + r) * 128:(bb * 4 + r + 1) * 128],
                             in_=src[:, i, r * 128:(r + 1) * 128], identity=ident_b[:])
                S.op("dve", "tensor_copy", reads=[PSTR], writes=[r_dstT],
                     out=dstT[:, :, half * 256:(half + 1) * 256].rearrange("p r (b t) -> p b r t", b=2),
                     in_=PST[:, :].rearrange("p (b r t) -> p b r t", b=2, r=4))

    def gate_j(og, j):
        mb = og % 2
        bsel = []
        for which in range(2):
            ws = wr_i[0] % NWR
            wr_i[0] += 1
            S.dma("pool", writes=[r_wring[ws]], out=wring[ws][:].rearrange("p c f -> p (c f)"),
                  in_=wfm[13 + which * 8 + j])
            b = nbank(0, 7)
            for c in range(8):
                mm(PS[b][:, :], wring[ws][:, c, :], xq[0][:, c, :], c == 0, c == 7,
                   reads=[r_wring[ws], r_xq[0]], writes=[PSR[b]])
            act(sg[which][:], PS[b][:, :], AF.Sigmoid, reads=[PSR[b]], writes=[r_sg[which]])
        for which, (wsb, srcT, r_srcT) in enumerate(((wpa_sb, aT, r_aT), (wpf_sb, fT, r_fT))):
            b = nbank(0, 7)
            bsel.append(b)
            for r in range(4):
                mm(PS[b][:, :], wsb[:, r, j * 128:(j + 1) * 128], srcT[:, r, :], r == 0, r == 3,
                   reads=[r_w3, r_srcT], writes=[PSR[b]])
        S.op("dve", "tensor_tensor", reads=[PSR[bsel[0]], r_sg[0]], writes=[r_sg[2]], out=sg[2][:],
             in0=PS[bsel[0]][:, :], in1=sg[0][:], op=ALU.mult)
        S.op("dve", "tensor_tensor", reads=[PSR[bsel[1]], r_sg[1]], writes=[r_sg[3]], out=sg[3][:],
             in0=PS[bsel[1]][:, :], in1=sg[1][:], op=ALU.mult)
        S.op("dve", "tensor_tensor", reads=[r_sg[2], r_sg[3]], writes=[r_mixT[mb][j]], out=mixT[mb][:, j, :],
             in0=sg[2][:], in1=sg[3][:], op=ALU.add)

    def tile_z(og, blk):
        mb = og % 2
        i = og * 4 + blk
        xs_ = 0
        S.dma("sp", writes=[r_xres[xs_]], out=xres[xs_][:], in_=xo[i * 128:(i + 1) * 128, :])
        for half in range(2):
            b = nbank(0, 7)
            for c in range(8):
                mm(PS[b][:, :], mixT[mb][:, c, blk * 128:(blk + 1) * 128], wout_sb[:, c, half * 512:(half + 1) * 512],
                   c == 0, c == 7, reads=[r_mixT[mb][c], r_w3], writes=[PSR[b]])
            S.op("dve", "scalar_tensor_tensor", reads=[PSR[b], r_xres[xs_]], writes=[r_h1[i]],
                 out=h1[:, i, half * 512:(half + 1) * 512], in0=xres[xs_][:, half * 512:(half + 1) * 512],
                 scalar=ALPHA, in1=PS[b][:, :], op0=ALU.mult, op1=ALU.add)
        if i > 0:
            route_pe(i - 1)
        layer_norm(h1[:, i, :], r_h1[i], h1[:, i, :], r_h1[i], g1[:], b1[:], r_w3)
        if debug:
            S.dma("sp", reads=[r_h1[i]], out=dbg_h1[i * 128:(i + 1) * 128, :], in_=h1[:, i, :])

    gate_prep(0)
    for j in range(8):
        gate_j(0, j)
    for og in range(4):
        if og + 1 < 4:
            gate_prep(og + 1)
        for blk in range(4):
            if og + 1 < 4:
                gate_j(og + 1, 2 * blk)
                gate_j(og + 1, 2 * blk + 1)
            tile_z(og, blk)

    route_pe(NOWN - 1)
    S.barrier()
    A.release(m_r)
    A.top = A.limit
    wdb = [A.alloc([128, 8, 1024], BF16) for _ in range(3)]
    r_wdb = [[Res() for _ in range(8)] for _ in range(3)]
    NWG = 8
    wgr = [A.alloc([128, 8, 128], BF16) for _ in range(NWG)]
    r_wgr = [Res() for _ in range(NWG)]
    bgu_sb = A.alloc([128, NE * 16], F32)
    r_w4 = Res("w4")
    S.dma("sp", writes=[r_w4], out=bgu_sb[:], in_=bgu)
    m_w = A.mark()
    wg_issued = [0]
    wd_issued = [0]

    def issue_wg(upto):
        while wg_issued[0] < min(upto, NE * 16):
            n_ = wg_issued[0]
            e_, j_, wh_ = n_ // 16, (n_ % 16) // 2, n_ % 2
            ws_ = n_ % NWG
            S.dma("pool", writes=[r_wgr[ws_]], out=wgr[ws_][:].rearrange("p c f -> p (c f)"),
                  in_=(wgu[e_, j_ + 8 * wh_] if e_ < E_PRE0 else wgu_b[e_ - E_PRE0, j_ + 8 * wh_]))
            wg_issued[0] += 1

    def issue_wd(upto_e):
        while wd_issued[0] < min(upto_e, NE):
            e_ = wd_issued[0]
            for kk in range(8):
                S.dma("pool", writes=[r_wdb[e_ % 3][kk]], out=wdb[e_ % 3][:, kk, :],
                      in_=(wd[e_, kk] if e_ < E_PRE0 else wd_b[e_ - E_PRE0, kk]))
            wd_issued[0] += 1

    issue_wg(NWG)
    issue_wd(1)
    ebase_sb = A.alloc([128, 32], F32)
    striu_b = A.alloc([128, 128], BF16)
    ones_b = A.alloc([128, 128], BF16)
    S.dma("sp", writes=[r_w4r], out=ebase_sb[:], in_=ebase)
    S.dma("pool", writes=[r_w4r], out=striu_b[:], in_=striu_d)
    S.op("pool", "memset", writes=[r_w4r], ap=ones_b[:], constant=1.0)
    top8a = A.alloc([128, NOWN, 8], F32)
    mska = A.alloc([128, NOWN, 32], F32)
    mskb = A.alloc([128, NOWN, 32], BF16)
    exa = A.alloc([128, NOWN, 32], F32)
    rka = A.alloc([128, NOWN, 32], F32)
    sfa = A.alloc([128, NOWN, 32], F32)
    ova = A.alloc([128, NOWN, 32], F32)
    nva = A.alloc([128, NOWN, 32], F32)
    oha = A.alloc([128, NOWN, 32], F32)
    t32a = A.alloc([128, NOWN, 32], F32)
    sma = A.alloc([128, NOWN], F32)
    rca = A.alloc([128, NOWN], F32)
    slots_f = A.alloc([128, NOWN, 4], F32)
    h1b = [A.alloc([128, 1024], BF16) for _ in range(2)]
    r_h1b = [Res(), Res()]
    r_rt = Res("router")

    def dv(name, reads=(), writes=(), **kw):
        S.op("dve", name, reads=[r_rt] + list(reads), writes=[r_rt] + list(writes), **kw)
    for i in range(NOWN):
        dv("max", reads=[r_lg[i]], out=top8a[:, i, :], in_=lgall[:, i, :])
    dv("tensor_tensor", out=mska[:], in0=lgall[:], in1=bc_last(top8a[:, :, 3], 32), op=ALU.is_ge)
    dv("tensor_copy", out=mskb[:], in_=mska[:])
    dv("tensor_tensor", out=exa[:], in0=lgall[:], in1=bc_last(top8a[:, :, 0], 32), op=ALU.subtract)
    act(exa[:], exa[:], AF.Exp, reads=[r_rt], writes=[r_rt])
    dv("tensor_tensor", out=exa[:], in0=exa[:], in1=mska[:], op=ALU.mult)
    dv("tensor_reduce", out=sma[:], in_=exa[:], axis=AX.X, op=ALU.add)
    dv("reciprocal", out=rca[:], in_=sma[:])
    rb = nbank(0, 7)
    for i in range(NOWN):
        for i2 in range(i):
            mm(PS[rb][:, i * 32:(i + 1) * 32], ones_b[:], mskb[:, i2, :], i2 == 0, False, reads=[r_rt, r_w4r],
               writes=[PSR[rb]], last=False)
        mm(PS[rb][:, i * 32:(i + 1) * 32], striu_b[:], mskb[:, i, :], i == 0, True, reads=[r_rt, r_w4r],
           writes=[PSR[rb]])
    dv("tensor_copy", reads=[PSR[rb]], out=rka[:].rearrange("p a b -> p (a b)"), in_=PS[rb][:, :])
    eb_ap = ebase_sb[:, :]
    ebase_bc = bass.AP(eb_ap.tensor, eb_ap.offset, [list(eb_ap.ap[0]), [0, NOWN], list(eb_ap.ap[1])])
    dv("tensor_scalar", out=ova[:], in0=rka[:], scalar1=float(CAP), scalar2=None, op0=ALU.is_ge)
    dv("tensor_tensor", reads=[r_w4r], out=sfa[:], in0=rka[:], in1=ebase_bc, op=ALU.add)
    dv("tensor_scalar", out=nva[:], in0=ova[:], scalar1=-1.0, scalar2=1.0, op0=ALU.mult, op1=ALU.add)
    dv("tensor_tensor", out=sfa[:], in0=sfa[:], in1=nva[:], op=ALU.mult)
    dv("scalar_tensor_tensor", out=sfa[:], in0=ova[:], scalar=float(NSLOT), in1=sfa[:], op0=ALU.mult, op1=ALU.add)
    dv("tensor_tensor", out=exa[:], in0=exa[:], in1=bc_last(rca[:, :], 32), op=ALU.mult)
    dv("tensor_tensor", writes=r_route, out=G[:], in0=exa[:], in1=nva[:], op=ALU.mult)
    for k in range(4):
        dv("tensor_tensor", out=oha[:], in0=lgall[:], in1=bc_last(top8a[:, :, k], 32), op=ALU.is_equal)
        dv("tensor_tensor", out=t32a[:], in0=oha[:], in1=sfa[:], op=ALU.mult)
        dv("tensor_reduce", out=slots_f[:, :, k], in_=t32a[:], axis=AX.X, op=ALU.add)
        dv("tensor_tensor", out=t32a[:], in0=oha[:], in1=G[:], op=ALU.mult)
        dv("tensor_reduce", writes=r_route, out=gk[:, :, k], in_=t32a[:], axis=AX.X, op=ALU.add)
    dv("tensor_copy", writes=r_route, out=slots_i[:], in_=slots_f[:])
    for i in range(NOWN):
        k2 = i % 2
        act(h1b[k2][:], h1[:, i, :], AF.Copy, reads=[r_h1[i]], writes=[r_h1b[k2]])
        for k in range(4):
            S.dma("pool", name="indirect_dma_start", reads=[r_h1b[k2], r_route[i]], writes=[],
                  out=xs_d[:, :], out_offset=bass.IndirectOffsetOnAxis(ap=slots_i[:, i, k:k + 1], axis=0),
                  in_=h1b[k2][:, :], in_offset=None, bounds_check=None, oob_is_err=False)
        if debug:
            S.dma("sp", reads=[r_route[i]], out=dbg_G[i * 128:(i + 1) * 128, :], in_=G[:, i, :])
        act(h1[:, i, :], h1[:, i, :], AF.Copy, reads=[r_h1[i]], writes=[r_h1[i]], scale=ALPHA)

    S.barrier()
    A.release(m_w)
    A.top = A.limit
    xrows = [A.alloc([128, CAP // 128, 1024], BF16) for _ in range(2)]
    r_xrows = [Res(), Res()]
    xsT = [A.alloc([128, 8, CAP], BF16) for _ in range(2)]
    r_xsT = [Res(), Res()]
    actT = [A.alloc([128, 8, CAP], BF16) for _ in range(2)]
    r_actT = [[Res() for _ in range(8)] for _ in range(2)]
    ystage = [A.alloc([128, 1024], F32) for _ in range(2)]
    r_ystage = [Res(), Res()]
    bdb = [A.alloc([128, 1024], F32) for _ in range(2)]
    r_bdb = [Res(), Res()]
    tg_ = [A.alloc([128, CAP], F32) for _ in range(2)]
    ts_ = [A.alloc([128, CAP], F32) for _ in range(2)]
    tu_ = [A.alloc([128, CAP], F32) for _ in range(2)]
    r_tg = [Res(), Res()]
    r_ts = [Res(), Res()]
    r_tu = [Res(), Res()]
    NST = CAP // 128
    wg_i = [0]
    tmp_i = [0]
    ys_i = [0]
    def load_xrows(e_):
        S.dma("sp", writes=[r_xrows[e_ % 2]], out=xrows[e_ % 2][:],
              in_=xs_d[e_ * CAP:(e_ + 1) * CAP, :].rearrange("(s p) f -> p s f", p=128))
    load_xrows(0)

    def exp_front(e):
        wb = e % 2
        if e + 1 < NE:
            load_xrows(e + 1)
        bd_row = bd[e:e + 1, :]
        S.dma("sp", writes=[r_bdb[wb]], out=bdb[wb][:],
              in_=bass.AP(bd_row.tensor, bd_row.offset, [[0, 128], [1, 1024]]))
        for st in range(NST):
            tb_, tr_ = (PST[:, :], PSTR) if st % 2 == 0 else (PS[6][:, :].bitcast(BF16), PSR[6])
            for c in range(8):
                S.op("pe", "transpose", reads=[r_xrows[wb], r_const], writes=[tr_], inc=(c == 7),
                     out=tb_[:, c * 128:(c + 1) * 128], in_=xrows[wb][:, st, c * 128:(c + 1) * 128],
                     identity=ident_b[:])
            act(xsT[wb][:, :, st * 128:(st + 1) * 128], tb_.rearrange("p (c t) -> p c t", c=8), AF.Copy,
                reads=[tr_], writes=[r_xsT[wb]])
        for j in range(8):
            n0 = e * 16 + j * 2
            issue_wg(n0 + 2)
            slots = [n0 % NWG, (n0 + 1) % NWG]
            bg = nbank(0, 6)
            for c in range(8):
                mm(PS[bg][:, 0:CAP], wgr[slots[0]][:, c, :], xsT[wb][:, c, :], c == 0, c == 7,
                   reads=[r_wgr[slots[0]], r_xsT[wb]], writes=[PSR[bg]])
            bu = nbank(0, 6)
            for c in range(8):
                mm(PS[bu][:, 0:CAP], wgr[slots[1]][:, c, :], xsT[wb][:, c, :], c == 0, c == 7,
                   reads=[r_wgr[slots[1]], r_xsT[wb]], writes=[PSR[bu]])
            k = tmp_i[0] % 2
            tmp_i[0] += 1
            colg = e * 16 + j
            colu = e * 16 + 8 + j
            S.op("dve", "tensor_scalar", reads=[PSR[bg], r_w4], writes=[r_tg[k]], out=tg_[k][:], in0=PS[bg][:, 0:CAP],
                 scalar1=bgu_sb[:, colg:colg + 1], scalar2=7.0, op0=ALU.add, op1=ALU.min)
            act(ts_[k][:], tg_[k][:], AF.Sigmoid, reads=[r_tg[k]], writes=[r_ts[k]], scale=1.702)
            act(tu_[k][:], PS[bu][:, 0:CAP], AF.Identity, reads=[PSR[bu], r_w4], writes=[r_tu[k]],
                bias=bgu_sb[:, colu:colu + 1])
            S.op("dve", "tensor_scalar", reads=[r_tu[k]], writes=[r_tu[k]], out=tu_[k][:], in0=tu_[k][:],
                 scalar1=7.0, scalar2=-7.0, op0=ALU.min, op1=ALU.max)
            S.op("dve", "scalar_tensor_tensor", reads=[r_tg[k], r_tu[k]], writes=[r_tu[k]], out=tu_[k][:], in0=tu_[k][:],
                 scalar=1.0, in1=tg_[k][:], op0=ALU.add, op1=ALU.mult)
            S.op("dve", "tensor_tensor", reads=[r_ts[k], r_tu[k]], writes=[r_actT[wb][j]],
                 out=actT[wb][:, j, :], in0=ts_[k][:], in1=tu_[k][:], op=ALU.mult)
        issue_wd(e + 1)

    def exp_back(e):
        wb = e % 2
        for st in range(NST):
            ysb = ys_i[0] % 2
            ys_i[0] += 1
            for half in range(2):
                b = nbank(0, 6)
                for kk in range(8):
                    mm(PS[b][:, :], actT[wb][:, kk, st * 128:(st + 1) * 128], wdb[e % 3][:, kk, half * 512:(half + 1) * 512],
                       kk == 0, kk == 7, reads=[r_actT[wb][kk], r_wdb[e % 3][kk]], writes=[PSR[b]])
                S.op("dve", "tensor_tensor", reads=[PSR[b], r_bdb[wb]], writes=[r_ystage[ysb]],
                     out=ystage[ysb][:, half * 512:(half + 1) * 512], in0=PS[b][:, :],
                     in1=bdb[wb][:, half * 512:(half + 1) * 512], op=ALU.add)
            r0 = e * CAP + st * 128
            S.dma("sp", reads=[r_ystage[ysb]], out=ys_d[r0:r0 + 128, :], in_=ystage[ysb][:])


    exp_front(0)
    for e in range(NE):
        if e + 1 < NE:
            exp_front(e + 1)
        exp_back(e)

    S.barrier()
    A.release(m_h1)
    g2 = A.alloc([128, 1024], F32)
    b2 = A.alloc([128, 1024], F32)
    S.dma("sp", writes=[r_w4], out=g2[:], in_=ln2g)
    S.dma("sp", writes=[r_w4], out=b2[:], in_=ln2b)
    lnt = A.alloc([128, 1024], F32)
    stat = A.alloc([128, 4, 8], F32)
    NYK = 4
    yk = [[A.alloc([128, 1024], F32) for _ in range(4)] for _ in range(NYK)]
    r_yk = [[Res() for _ in range(4)] for _ in range(NYK)]
    for a_ in range(NYK):
        for k in range(4):
            S.op("dve", "memset", writes=[r_yk[a_][k]], ap=yk[a_][k][:], constant=0.0)
    def gathers(i):
        a_ = i % NYK
        for k in range(4):
            S.dma("pool", name="indirect_dma_start", reads=[r_route[i]], writes=[r_yk[a_][k]],
                  out=yk[a_][k][:, :], out_offset=None, in_=ys_d[:, :],
                  in_offset=bass.IndirectOffsetOnAxis(ap=slots_i[:, i, k:k + 1], axis=0),
                  bounds_check=None, oob_is_err=False)

    def stt(i, k):
        a_ = i % NYK
        S.op("dve", "scalar_tensor_tensor", reads=[r_yk[a_][k], r_route[i], r_h1[i]], writes=[r_h1[i]],
             out=h1[:, i, :], in0=yk[a_][k][:], scalar=gk[:, i, k:k + 1], in1=h1[:, i, :],
             op0=ALU.mult, op1=ALU.add)

    for i in range(min(NYK - 1, NOWN)):
        gathers(i)
    for i in range(NOWN + 1):
        if i + NYK - 1 < NOWN:
            gathers(i + NYK - 1)
        st_prev = None
        if i >= 1:
            st_prev = ln_stages(h1[:, i - 1, :], r_h1[i - 1], h1[:, i - 1, :], r_h1[i - 1], g2[:], b2[:], r_w4)
            st_prev[0]()
        for k in range(4):
            if i < NOWN:
                stt(i, k)
            if st_prev is not None:
                st_prev[k + 1]()
        if i >= 1:
            S.dma("sp", reads=[r_h1[i - 1]], out=out[(i - 1) * 128:i * 128, :], in_=h1[:, i - 1, :])
    S.barrier()
    S.emit()
    return nc


def _prep(inp):
    f = np.float32
    x = np.asarray(inp["x"], f)
    w_in = np.asarray(inp["w_in"], f)[0]
    sh = {}

    def fm(cols):
        return np.ascontiguousarray(w_in[:, cols].reshape(8, 128, 128).transpose(1, 0, 2).reshape(128, 1024))
    pieces = []
    for j in range(4):
        pieces.append(fm(np.arange(C_FOXK + 128 * j, C_FOXK + 128 * (j + 1))))
    pieces.append(fm(np.arange(C_SWAK, C_SWAK + 128)))
    for j in range(4):
        pieces.append(fm(np.arange(C_FOXQ + 128 * j, C_FOXQ + 128 * (j + 1))))
    for r in range(4):
        cols = np.concatenate([np.arange(C_SWAQ + r * 64, C_SWAQ + (r + 1) * 64),
                               np.arange(C_SWAQ + (4 + r) * 64, C_SWAQ + (5 + r) * 64)])
        pieces.append(fm(cols))
    for j in range(8):
        pieces.append(fm(np.arange(C_GA + 128 * j, C_GA + 128 * (j + 1))))
    for j in range(8):
        pieces.append(fm(np.arange(C_GF + 128 * j, C_GF + 128 * (j + 1))))
    sh["wfm"] = np.stack(pieces)
    cols = np.concatenate([np.arange(C_FOXV, C_FOXV + 512), np.arange(C_FL, C_FL + 8), np.arange(C_SWAV, C_SWAV + 128)])
    sh["wtm"] = np.ascontiguousarray(w_in[:, cols].reshape(8, 128, 648).transpose(1, 0, 2))
    sh["wpa"] = np.ascontiguousarray(np.asarray(inp["w_proj_swa"], f)[0].reshape(4, 128, 1024).transpose(1, 0, 2))
    sh["wpf"] = np.ascontiguousarray(np.asarray(inp["w_proj_fox"], f)[0].reshape(4, 128, 1024).transpose(1, 0, 2))
    sh["wout"] = np.ascontiguousarray(np.asarray(inp["w_out"], f)[0].reshape(8, 128, 1024).transpose(1, 0, 2))
    rep = lambda v, n=128: np.ascontiguousarray(np.broadcast_to(np.asarray(v, f).reshape(1, -1), (n, np.asarray(v).size)))
    sh["ln1g"] = rep(inp["ln1_g"][0])
    sh["ln1b"] = rep(inp["ln1_b"][0])
    sh["ln2g"] = rep(inp["ln2_g"][0])
    sh["ln2b"] = rep(inp["ln2_b"][0])
    sh["wr"] = np.ascontiguousarray(np.asarray(inp["w_router"], f)[0].reshape(8, 128, 32).transpose(1, 0, 2))
    sh["br"] = rep(inp["b_router"][0])
    sh["bfr"] = rep(np.tile(np.asarray(inp["b_forget"], f)[0], NB))
    sh["sink8"] = rep(np.asarray(inp["sink"], f)[0].reshape(-1))
    sh["ident"] = np.eye(128, dtype=f)
    sh["triu"] = np.triu(np.ones((128, 128), f))
    wguf = np.asarray(inp["w_gate_up"], f)[0]
    sh["wgu"] = np.ascontiguousarray(wguf.reshape(NE, 8, 128, 16, 128).transpose(0, 3, 2, 1, 4).reshape(NE, 16, 128, 1024))
    sh["wd"] = np.ascontiguousarray(np.asarray(inp["w_down"], f)[0].reshape(NE, 8, 128, 1024))
    bguf = np.asarray(inp["b_gate_up"], f)[0]
    sh["bgu"] = np.ascontiguousarray(bguf.reshape(NE, 16, 128).transpose(2, 0, 1).reshape(128, NE * 16))
    sh["bd"] = np.ascontiguousarray(np.asarray(inp["b_down"], f)[0])
    sh["ebase"] = np.ascontiguousarray(np.broadcast_to((np.arange(NE, dtype=f) * CAP).reshape(1, NE), (128, NE)))
    sh["striu"] = np.triu(np.ones((128, 128), f), 1)

    slopes = (2.0 ** (-8.0 * np.arange(1, 9) / 8)).astype(np.float64)
    kk = np.arange(128)[:, None]
    qq = np.arange(128)[None, :]
    tb = np.full((128, 3, 2, 4, 128), NEG, np.float64)
    for g in range(2):
        for r in range(4):
            sl = slopes[4 * g + r]
            dist = qq - kk
            tb[:, 0, g, r, :] = np.where(dist >= 0, -sl * dist, NEG)
            dist = qq + 128 - kk
            tb[:, 1, g, r, :] = np.where(dist < 128, -sl * dist, NEG)
    tri = (kk <= qq).astype(f)

    percore = []
    own_all = []
    for c in range(8):
        b, p = c // 2, c % 2
        own = [I for I in range(NB) if ((I % 4 in (0, 3)) == (p == 0))]
        own_all.append(own)
        xb = x[b]
        xT = xb.T
        d = {}
        d["xT3"] = np.ascontiguousarray(xT.reshape(8, 128, 4096).transpose(1, 0, 2))
        tok_own = np.concatenate([np.arange(I * 128, (I + 1) * 128) for I in own])
        d["xTo3"] = np.ascontiguousarray(xT[:, tok_own].reshape(8, 128, 2048).transpose(1, 0, 2))
        d["xo"] = np.ascontiguousarray(xb[tok_own])
        xw = np.zeros((1024, 4096), f)
        for i, I in enumerate(own):
            if I > 0:
                xw[:, (2 * i) * 128:(2 * i + 1) * 128] = xT[:, (I - 1) * 128:I * 128]
            xw[:, (2 * i + 1) * 128:(2 * i + 2) * 128] = xT[:, I * 128:(I + 1) * 128]
        d["xTw3"] = np.ascontiguousarray(xw.reshape(8, 128, 4096).transpose(1, 0, 2))
        t = tb.copy()
        t[:, 2] = t[:, 1] if own[0] > 0 else NEG
        d["swatbl"] = np.ascontiguousarray(t.reshape(128, -1).astype(f))
        mp = np.zeros((128, 16, 2, 128), f)
        pr = np.zeros((128, 16), f)
        for i, I in enumerate(own):
            if I == 2 * i:
                mp[:, i, 0, :] = tri
                mp[:, i, 1, :] = 0.0
            else:
                assert I == 2 * i + 1
                mp[:, i, 0, :] = 1.0
                mp[:, i, 1, :] = tri
                pr[:, i] = 1.0
        d["maskpair"] = np.ascontiguousarray(mp.reshape(128, -1))
        d["par"] = pr
        d.update(sh)
        percore.append(d)
    return percore, own_all


_NC_CACHE = {}


def kernel(**inputs):
    percore, own_all = _prep(inputs)
    if "nc" not in _NC_CACHE:
        _NC_CACHE["nc"] = build_nc()
    nc = _NC_CACHE["nc"]
    res = run_bass_kernel_spmd(nc, percore, core_ids=list(range(8)))
    outp = np.zeros((4, 4096, 1024), np.float32)
    for c in range(8):
        o = np.asarray(res.results[c]["out"], np.float32)
        for i, I in enumerate(own_all[c]):
            outp[c // 2, I * 128:(I + 1) * 128, :] = o[i * 128:(i + 1) * 128]
    return outp
```

```python
import contextlib
import numpy as np
import concourse.bass as bass
import concourse.mybir as mybir
from concourse.bass_utils import run_bass_kernel_spmd

F32 = mybir.dt.float32
BF16 = mybir.dt.bfloat16
AF = mybir.ActivationFunctionType
ALU = mybir.AluOpType
AX = mybir.AxisListType

NB = 32
NOWN = 16
NE = 32
ALPHA = float(2.0 ** 0.25)
EPS = 1e-5
NEG = -30000.0
CAP = 384
NSLOT = NE * CAP
N_PRE = 8
E_PRE0 = NE - N_PRE
C_SWAQ, C_SWAK, C_SWAV, C_FOXQ, C_FOXK, C_FOXV, C_FL, C_GA, C_GF = 0, 512, 640, 768, 1280, 1792, 2304, 2312, 3336


class Res:
    __slots__ = ("name", "last_w", "readers")

    def __init__(self, name=""):
        self.name = name
        self.last_w = None
        self.readers = {}


class Sched:
    NDMA = 96
    NSP = 32

    def __init__(self, nc, same_engine_wait=True):
        self.nc = nc
        self.engs = ["pe", "act", "dve", "pool", "sp"]
        self.prog = {k: [] for k in self.engs}
        self.cnt = {k: 0 for k in self.engs}
        self.waited = {k: {} for k in self.engs}
        self.dma_i = {"sp": 0, "pool": 0, "act": 0}
        self.dma_cnt = [0] * self.NDMA
        self.same_engine_wait = same_engine_wait

    def _deps(self, reads, writes):
        deps = {}

        def add(k, v):
            if deps.get(k, 0) < v:
                deps[k] = v
        for r in reads:
            if r.last_w is not None:
                add(*r.last_w)
        for w in writes:
            if w.last_w is not None:
                add(*w.last_w)
            for k, v in w.readers.items():
                add(k, v)
        return deps

    def _filter(self, eng, deps):
        out = []
        wd = self.waited[eng]
        for k, v in deps.items():
            if k == eng and (not self.same_engine_wait or eng in ("pe", "sp")):
                continue
            if wd.get(k, 0) >= v:
                continue
            wd[k] = v
            out.append((k, v))
        return out

    def _mark(self, tok, reads, writes):
        for w in writes:
            w.last_w = tok
            w.readers = {}
        k, v = tok
        for r in reads:
            if r in writes:
                continue
            if r.readers.get(k, 0) < v:
                r.readers[k] = v

    def op(self, eng, name, reads=(), writes=(), inc=True, **kw):
        waits = self._filter(eng, self._deps(reads, writes))
        if inc:
            self.cnt[eng] += 1
            tok = (eng, self.cnt[eng])
        else:
            tok = (eng, self.cnt[eng] + 1)
        self.prog[eng].append((waits, name, kw, eng if inc else None))
        self._mark(tok, reads, writes)
        return tok

    def dma(self, qeng, reads=(), writes=(), name="dma_start", **kw):
        deps = self._deps(reads, writes)
        if qeng == "sp":
            i = self.dma_i[qeng]
            self.dma_i[qeng] = (self.dma_i[qeng] + 1) % self.NSP
        else:
            i = self.NSP + self.dma_i[qeng]
            self.dma_i[qeng] = (self.dma_i[qeng] + 1) % (self.NDMA - self.NSP)
        if self.dma_cnt[i] > 0:
            deps[("d", i)] = max(deps.get(("d", i), 0), self.dma_cnt[i])
        waits = self._filter(qeng, deps)
        self.dma_cnt[i] += 16
        tok = (("d", i), self.dma_cnt[i])
        self.prog[qeng].append((waits, name, kw, ("d", i)))
        self._mark(tok, reads, writes)
        return tok

    def barrier(self):
        deps = {k: self.cnt[k] for k in self.engs if self.cnt[k] > 0}
        for i in range(self.NDMA):
            if self.dma_cnt[i] > 0:
                deps[("d", i)] = self.dma_cnt[i]
        for eng in self.engs:
            d = {k: v for k, v in deps.items() if k != eng}
            waits = self._filter(eng, d)
            self.prog[eng].append((waits, None, None, None))

    def emit(self):
        nc = self.nc
        with contextlib.ExitStack() as st:
            sems = {}
            for k in self.engs:
                sems[k] = st.enter_context(nc.semaphore("s_" + k))
            for i in range(self.NDMA):
                sems[("d", i)] = st.enter_context(nc.semaphore("s_d%d" % i))
            block = st.enter_context(nc.Block())

            def run(name, e):
                for waits, op, kw, inc in self.prog[name]:
                    for k, v in waits:
                        e.wait_ge(sems[k], v)
                    if op is None:
                        continue
                    ins = getattr(e, op)(**kw)
                    if inc is None:
                        continue
                    ins.then_inc(sems[inc], 16 if isinstance(inc, tuple) else 1)

            block.tensor(lambda e: run("pe", e))
            block.scalar(lambda e: run("act", e))
            block.vector(lambda e: run("dve", e))
            block.gpsimd(lambda e: run("pool", e))
            block.sync(lambda e: run("sp", e))


class Arena:
    def __init__(self, nc, base=16512, limit=16512 + 207 * 1024):
        self.nc = nc
        self.off = base
        self.limit = limit
        self.top = limit
        self.n = 0

    def alloc_top(self, shape, dtype):
        esz = 2 if dtype == BF16 else 4
        per = esz * int(np.prod(shape[1:]))
        per = (per + 63) // 64 * 64
        self.top -= per
        assert self.off <= self.top, ("SBUF overflow", self.off, self.top)
        self.n += 1
        return self.nc.alloc_sbuf_tensor_at("t%d" % self.n, list(shape), dtype, offset=self.top)

    def alloc(self, shape, dtype):
        esz = 2 if dtype == BF16 else 4
        per = esz * int(np.prod(shape[1:]))
        per = (per + 63) // 64 * 64
        off = self.off
        self.off += per
        assert self.off <= self.top, ("SBUF overflow", self.off, self.top)
        self.n += 1
        return self.nc.alloc_sbuf_tensor_at("t%d" % self.n, list(shape), dtype, offset=off)

    def mark(self):
        return self.off

    def release(self, m):
        self.off = m


def bc_last(ap, n):
    return bass.AP(ap.tensor, ap.offset, [list(x) for x in ap.ap] + [[0, n]])


def midbc(ap2, n, step=0):
    return bass.AP(ap2.tensor, ap2.offset, [list(ap2.ap[0]), [step, n], list(ap2.ap[1])])


def build_nc(debug=False):
    nc = bass.Bass("TRN2", target_bir_lowering=False)

    def din(name, shape):
        return nc.dram_tensor(name, list(shape), F32, kind="ExternalInput").ap()
    xT3 = din("xT3", [128, 8, 4096])
    xTw3 = din("xTw3", [128, 8, 4096])
    xTo3 = din("xTo3", [128, 8, 2048])
    xo = din("xo", [2048, 1024])
    wfm = din("wfm", [29, 128, 1024])
    wtm = din("wtm", [128, 8, 648])
    wpa = din("wpa", [128, 4, 1024])
    wpf = din("wpf", [128, 4, 1024])
    wout = din("wout", [128, 8, 1024])
    ln1g = din("ln1g", [128, 1024])
    ln1b = din("ln1b", [128, 1024])
    ln2g = din("ln2g", [128, 1024])
    ln2b = din("ln2b", [128, 1024])
    wr = din("wr", [128, 8, 32])
    br = din("br", [128, 32])
    bfr = din("bfr", [128, 256])
    sink8 = din("sink8", [128, 8])
    swatbl = din("swatbl", [128, 3 * 2 * 4 * 128])
    maskpair = din("maskpair", [128, 16 * 2 * 128])
    par = din("par", [128, 16])
    ident_d = din("ident", [128, 128])
    triu_d = din("triu", [128, 128])
    wgu = din("wgu", [NE, 16, 128, 1024])
    wd = din("wd", [NE, 8, 128, 1024])
    bgu = din("bgu", [128, NE * 16])
    bd = din("bd", [NE, 1024])
    ebase = din("ebase", [128, 32])
    striu_d = din("striu", [128, 128])
    xs_d = nc.dram_tensor("xs_scratch", [NSLOT + 128, 1024], BF16, kind="Internal").ap()
    ys_d = nc.dram_tensor("ys_scratch", [NSLOT + 128, 1024], F32, kind="Internal").ap()
    r_xs = Res("xs")
    wgu_b = nc.dram_tensor("wgu_bf16", [N_PRE, 16, 128, 1024], BF16, kind="Internal").ap()
    wd_b = nc.dram_tensor("wd_bf16", [N_PRE, 8, 128, 1024], BF16, kind="Internal").ap()
    out = nc.dram_tensor("out", [2048, 1024], F32, kind="ExternalOutput").ap()
    if debug:
        dbg_h1 = nc.dram_tensor("dbg_h1", [2048, 1024], F32, kind="ExternalOutput").ap()
        dbg_G = nc.dram_tensor("dbg_G", [2048, 32], F32, kind="ExternalOutput").ap()
        dbg_swa = nc.dram_tensor("dbg_swa", [2048, 512], F32, kind="ExternalOutput").ap()
        dbg_fox = nc.dram_tensor("dbg_fox", [2048, 512], F32, kind="ExternalOutput").ap()

    S = Sched(nc)
    A = Arena(nc)

    PS = [nc.alloc_psum_tensor("ps%d" % i, [128, 512], F32) for i in range(7)]
    PSR = [Res("ps%d" % i) for i in range(7)]
    PST = nc.alloc_psum_tensor("pst", [128, 1024], BF16)
    PSTR = Res("pst")

    def mm(out_, lhsT, rhs, start, stop, reads, writes, last=None):
        S.op("pe", "matmul", reads=reads, writes=writes, inc=(stop if last is None else last),
             out=out_, lhsT=lhsT, rhs=rhs, start=start, stop=stop)

    def act(out_, in_, func, reads, writes, **kw):
        S.op("act", "activation", reads=reads, writes=writes, out=out_, in_=in_, func=func, **kw)

    ident_f = A.alloc([128, 128], F32)
    ident_b = A.alloc([128, 128], BF16)
    ones_f = A.alloc([128, 128], F32)
    r_const = Res("const")
    S.dma("sp", writes=[r_const], out=ident_f[:], in_=ident_d)
    S.dma("pool", writes=[r_const], out=ident_b[:], in_=ident_d)
    S.op("dve", "memset", writes=[r_const], ap=ones_f[:], constant=1.0)

    swa_out = A.alloc_top([128, NOWN, 512], BF16)
    fox_out = A.alloc_top([128, NOWN, 512], BF16)
    r_swa_out = [Res() for _ in range(NOWN)]
    r_fox_out = [Res() for _ in range(NOWN)]
    m_attn = A.mark()

    KTf = A.alloc([128, 4, 4096], BF16)
    QTf = A.alloc([128, 4, 2048], BF16)
    Vf = A.alloc([128, NB, 8, 65], BF16)
    KTs = A.alloc([128, 4096], BF16)
    QTs = A.alloc([128, 4, 2048], BF16)
    Vs = A.alloc([128, NB, 2, 65], BF16)
    r_KTf = [[Res() for _ in range(8)] for _ in range(4)]
    r_QTf = [[Res() for _ in range(4)] for _ in range(4)]
    r_Vf = [Res() for _ in range(NB)]
    r_KTs = [Res() for _ in range(8)]
    r_QTs = [[Res() for _ in range(4)] for _ in range(4)]
    r_Vs = [Res() for _ in range(NB)]
    zt = A.alloc([128, 256], F32)
    lt = A.alloc([128, 256], F32)
    cw = A.alloc([128, 256], F32)
    Tb = A.alloc([128, 256], F32)
    cend = A.alloc([128, 256], F32)
    cpos = A.alloc([128, 256], F32)
    cendo = A.alloc([128, NOWN, 8], F32)
    kscA = A.alloc([128, NB, 8], F32)
    kscB = A.alloc([128, 8, 2, 8], F32)
    ub = A.alloc([128, NOWN, 8, 8], F32)
    ubl = A.alloc([128, NOWN, 8], F32)
    bfr_sb = A.alloc([128, 256], F32)
    par_sb = A.alloc([128, 16], F32)
    triu_f = A.alloc([128, 128], F32)
    esink = A.alloc([128, 8], F32)
    r_fl = Res("fl")
    m_p12 = A.mark()

    xg = [A.alloc([128, 8, 512], BF16) for _ in range(2)]
    r_xg = [Res() for _ in range(2)]
    wres = A.alloc([128, 8, 1024], BF16)
    r_wres = Res("wres")
    wtm_sb = A.alloc([128, 8, 648], BF16)
    r_wtm = Res("wtm")

    S.dma("pool", writes=[r_wres], out=wres[:, 0:5, :], in_=wfm[0:5].rearrange("n p f -> p n f"))
    S.dma("pool", writes=[r_wtm], out=wtm_sb[:], in_=wtm)
    S.dma("sp", writes=[r_fl], out=bfr_sb[:], in_=bfr)
    S.dma("sp", writes=[r_fl], out=par_sb[:], in_=par)
    S.dma("sp", writes=[r_fl], out=triu_f[:], in_=triu_d)
    S.dma("sp", writes=[r_fl], out=esink[:], in_=sink8)
    S.op("dve", "memset", writes=r_Vf, ap=Vf[:, :, :, 64:65], constant=1.0)
    S.op("dve", "memset", writes=r_Vs, ap=Vs[:, :, :, 64:65], constant=1.0)

    def wpiece(n):
        return wres[:, (n if n < 5 else n - 5), :].rearrange("p (c f) -> p c f", c=8)

    bank_i = [0]

    def nbank(lo=0, hi=6):
        b = lo + bank_i[0] % (hi - lo)
        bank_i[0] += 1
        return b

    FLB = 6
    xg_i = [0]

    def load_xg(src):
        s = xg_i[0] % 2
        xg_i[0] += 1
        S.dma("pool", writes=[r_xg[s]], out=xg[s][:], in_=src)
        return s

    def proj_fm(s, piece, dst, r_dst, scale):
        b = nbank()
        wp = wpiece(piece)
        for c in range(8):
            mm(PS[b][:, :], wp[:, c, :], xg[s][:, c, :], c == 0, c == 7,
               reads=[r_wres, r_xg[s]], writes=[PSR[b]])
        act(dst, PS[b][:, :], AF.Copy, reads=[PSR[b]], writes=[r_dst], scale=scale)

    for g in range(8):
        s = load_xg(xT3[:, :, g * 512:(g + 1) * 512])
        for j in range(4):
            proj_fm(s, j, KTf[:, j, g * 512:(g + 1) * 512], r_KTf[j][g], 1.0)
        for blk in range(4):
            J = 4 * g + blk
            b = nbank()
            for c in range(8):
                mm(PS[b][:, :], xg[s][:, c, blk * 128:(blk + 1) * 128], wtm_sb[:, c, 0:512], c == 0, c == 7,
                   reads=[r_wtm, r_xg[s]], writes=[PSR[b]])
            S.op("dve", "tensor_copy", reads=[PSR[b]], writes=[r_Vf[J]],
                 out=Vf[:, J, :, 0:64], in_=PS[b][:, :].rearrange("p (h d) -> p h d", h=8))
            for c in range(8):
                mm(PS[FLB][:, J * 8:(J + 1) * 8], xg[s][:, c, blk * 128:(blk + 1) * 128], wtm_sb[:, c, 512:520],
                   c == 0, c == 7, reads=[r_wtm, r_xg[s]], writes=[PSR[FLB]])
    S.op("dve", "tensor_tensor", reads=[PSR[FLB], r_fl], writes=[r_fl], out=zt[:], in0=PS[FLB][:, 0:256],
         in1=bfr_sb[:], op=ALU.add)
    act(lt[:], zt[:], AF.Exp, reads=[r_fl], writes=[r_fl], scale=-1.0)
    act(lt[:], lt[:], AF.Ln, reads=[r_fl], writes=[r_fl], bias=1.0)
    act(esink[:], esink[:], AF.Exp, reads=[r_fl], writes=[r_fl])
    mm(PS[0][:, 0:256], triu_f[:], lt[:], True, True, reads=[r_fl], writes=[PSR[0]])
    mm(PS[1][:, 0:256], ones_f[:], lt[:], True, True, reads=[r_fl, r_const], writes=[PSR[1]])
    S.op("dve", "tensor_copy", reads=[PSR[0]], writes=[r_fl], out=cw[:], in_=PS[0][:, 0:256])
    S.op("dve", "tensor_copy", reads=[PSR[1]], writes=[r_fl], out=Tb[:], in_=PS[1][:, 0:256])
    Tb3 = Tb[:].rearrange("p (j h) -> p j h", h=8)
    cend3 = cend[:].rearrange("p (j h) -> p j h", h=8)
    cpos3 = cpos[:].rearrange("p (j h) -> p j h", h=8)
    for h in range(8):
        S.op("dve", "tensor_tensor_scan", reads=[r_fl, r_const], writes=[r_fl], out=cend3[:, :, h],
             data0=ones_f[:, 0:32], data1=Tb3[:, :, h], initial=0.0, op0=ALU.mult, op1=ALU.add)
    S.op("dve", "tensor_tensor", reads=[r_fl], writes=[r_fl], out=cpos[:], in0=cend[:], in1=Tb[:], op=ALU.subtract)
    S.op("dve", "tensor_tensor", reads=[r_fl], writes=[r_fl], out=cpos[:], in0=cpos[:], in1=cw[:], op=ALU.add)
    for i in range(NOWN):
        S.op("dve", "scalar_tensor_tensor", reads=[r_fl], writes=[r_fl], out=cendo[:, i, :], in0=Tb3[:, 2 * i + 1, :],
             scalar=par_sb[:, i:i + 1], in1=cend3[:, 2 * i, :], op0=ALU.mult, op1=ALU.add)

    for m_ in range(8):
        S.op("dve", "tensor_tensor", reads=[r_fl], writes=[r_fl], out=kscA[:, 4 * m_:4 * m_ + 4, :],
             in0=cpos3[:, 4 * m_:4 * m_ + 4, :], in1=midbc(cend3[:, 4 * m_ + 3, :], 4), op=ALU.subtract)
    act(kscA[:], kscA[:], AF.Exp, reads=[r_fl], writes=[r_fl])
    for ie in range(8):
        i_ = 2 * ie
        S.op("dve", "tensor_tensor", reads=[r_fl], writes=[r_fl], out=kscB[:, ie, :, :],
             in0=cpos3[:, 2 * i_:2 * i_ + 2, :], in1=midbc(cend3[:, 2 * i_ + 1, :], 2), op=ALU.subtract)
    act(kscB[:], kscB[:], AF.Exp, reads=[r_fl], writes=[r_fl])
    for i_ in range(NOWN):
        nfull = (2 * i_ + 2) // 4
        if nfull > 0:
            S.op("dve", "tensor_tensor", reads=[r_fl], writes=[r_fl], out=ub[:, i_, 0:nfull, :],
                 in0=midbc(cend3[:, 3, :], nfull, step=32), in1=midbc(cendo[:, i_, :], nfull), op=ALU.subtract)
    S.op("dve", "tensor_tensor", reads=[r_fl], writes=[r_fl], out=ubl[:], in0=midbc(cend3[:, 1, :], NOWN, step=16),
         in1=cendo[:], op=ALU.subtract)

    for g in range(8):
        s = load_xg(xTw3[:, :, g * 512:(g + 1) * 512])
        proj_fm(s, 4, KTs[:, g * 512:(g + 1) * 512], r_KTs[g], 1.0)
        for blk in range(4):
            Jw = 4 * g + blk
            b = nbank()
            for c in range(8):
                mm(PS[b][:, 0:128], xg[s][:, c, blk * 128:(blk + 1) * 128], wtm_sb[:, c, 520:648], c == 0, c == 7,
                   reads=[r_wtm, r_xg[s]], writes=[PSR[b]])
            S.op("dve", "tensor_copy", reads=[PSR[b]], writes=[r_Vs[Jw]],
                 out=Vs[:, Jw, :, 0:64], in_=PS[b][:, 0:128].rearrange("p (h d) -> p h d", h=2))
    S.dma("pool", reads=[], writes=[r_wres], out=wres[:, 0:4, :], in_=wfm[5:9].rearrange("n p f -> p n f"))
    S.dma("pool", reads=[], writes=[r_wres], out=wres[:, 4:8, :], in_=wfm[9:13].rearrange("n p f -> p n f"))
    for og in range(4):
        s = load_xg(xTo3[:, :, og * 512:(og + 1) * 512])
        for j in range(4):
            proj_fm(s, 5 + j, QTf[:, j, og * 512:(og + 1) * 512], r_QTf[j][og], 0.125)
        for r in range(4):
            proj_fm(s, 9 + r, QTs[:, r, og * 512:(og + 1) * 512], r_QTs[r][og], 0.125)

    S.barrier()
    A.release(m_p12)
    tbl = A.alloc([128, 3, 2, 512], F32)
    mpair = A.alloc([128, 16, 2, 128], BF16)
    PT = [A.alloc([128, 512], BF16) for _ in range(5)]
    r_PT = [Res() for _ in range(5)]
    r_PTs = [[Res() for _ in range(4)] for _ in range(5)]
    Stmp = [A.alloc([128, 512], F32) for _ in range(4)]
    r_Stmp = [Res() for _ in range(4)]
    rden = A.alloc([128, 2, 8], F32)
    r_rden = [Res(), Res()]
    r_tbl = Res("tbl")
    r_bias = Res("bias")
    S.dma("sp", writes=[r_tbl], out=tbl[:].rearrange("p a b c -> p (a b c)"), in_=swatbl)
    S.dma("pool", writes=[r_tbl], out=mpair[:].rearrange("p a b c -> p (a b c)"), in_=maskpair)
    zt_b = A.alloc([128, 2048], BF16)
    r_zt = Res("zt")
    S.op("pool", "memset", writes=[r_zt], ap=zt_b[:], constant=0.0)
    xs_flat = xs_d[0:NSLOT, :].rearrange("(n p r) f -> n p (r f)", p=128, r=2)
    for n_ in range(NSLOT // 256):
        S.dma("sp", reads=[r_zt], writes=[], out=xs_flat[n_], in_=zt_b[:])
    S.dma("pool", reads=[r_zt], writes=[], out=ys_d[NSLOT:NSLOT + 128, :], in_=zt_b[:, 0:1024])
    for pe_ in range(N_PRE):
        for j_ in range(16):
            S.dma("pool", out=wgu_b[pe_, j_], in_=wgu[E_PRE0 + pe_, j_])
        for k_ in range(8):
            S.dma("pool", out=wd_b[pe_, k_], in_=wd[E_PRE0 + pe_, k_])
    OB = [4, 5, 6]
    fin_i = [0]
    swa_it = [(i, g) for i in range(NOWN) for g in range(2)]

    def swa_S(n):
        i, g = swa_it[n]
        og = i // 4
        for w in range(2):
            b = (2 * n + w) % 4
            Jw = 2 * i + w
            mm(PS[b][:, :].rearrange("p (r q) -> p r q", r=4), KTs[g * 64:(g + 1) * 64, Jw * 128:(Jw + 1) * 128],
               QTs[g * 64:(g + 1) * 64, :, i * 128:(i + 1) * 128], True, True,
               reads=[r_KTs[Jw // 4]] + [r_QTs[r][og] for r in range(4)], writes=[PSR[b]])
            t = 0 if w == 1 else (2 if i == 0 else 1)
            S.op("dve", "tensor_tensor", reads=[PSR[b], r_tbl], writes=[r_Stmp[b]], out=Stmp[b][:],
                 in0=PS[b][:, :], in1=tbl[:, t, g, :], op=ALU.add)
            act(PT[b][:], Stmp[b][:], AF.Exp, reads=[r_Stmp[b]], writes=[r_PT[b]])

    def swa_PV(n):
        i, g = swa_it[n]
        ob = OB[n % 3]
        O3 = PS[ob][:, :].rearrange("p (r q) -> p r q", r=4)
        for r in range(4):
            for w in range(2):
                Jw = 2 * i + w
                pb = (2 * n + w) % 4
                mm(O3[:, r, 0:65], PT[pb][:, r * 128:(r + 1) * 128], Vs[:, Jw, g, :], w == 0, w == 1,
                   reads=[r_PT[pb], r_Vs[Jw]], writes=[PSR[ob]], last=(w == 1 and r == 3))
        k = fin_i[0] % 2
        fin_i[0] += 1
        S.op("dve", "tensor_tensor", reads=[PSR[ob], r_fl], writes=[r_rden[k]], out=rden[:, k, 0:4],
             in0=O3[:, :, 64], in1=esink[:, 4 * g:4 * g + 4], op=ALU.add)
        S.op("dve", "reciprocal", reads=[r_rden[k]], writes=[r_rden[k]], out=rden[:, k, 0:4], in_=rden[:, k, 0:4])
        for r in range(4):
            h = 4 * g + r
            S.op("dve", "tensor_scalar", reads=[PSR[ob], r_rden[k]], writes=[r_swa_out[i]],
                 out=swa_out[:, i, h * 64:(h + 1) * 64], in0=O3[:, r, 0:64], scalar1=rden[:, k, r:r + 1],
                 scalar2=None, op0=ALU.mult)

    swa_S(0)
    for n in range(len(swa_it)):
        if n + 1 < len(swa_it):
            swa_S(n + 1)
        swa_PV(n)

    chunks = []
    for i in range(NOWN):
        nj = 2 * i + 2
        for h in range(8):
            for j0 in range(0, nj, 4):
                chunks.append((i, h, list(range(j0, min(j0 + 4, nj)))))

    def fox_S(n):
        i, h, Js = chunks[n]
        b = n % 5
        j, hp = h // 2, h % 2
        for s_, J in enumerate(Js):
            mm(PS[b][:, s_ * 128:(s_ + 1) * 128], KTf[hp * 64:(hp + 1) * 64, j, J * 128:(J + 1) * 128],
               QTf[hp * 64:(hp + 1) * 64, j, i * 128:(i + 1) * 128], True, True,
               reads=[r_KTf[j][J // 4], r_QTf[j][i // 4]], writes=[PSR[b]], last=(s_ == len(Js) - 1))

    def fox_PV(n):
        i, h, Js = chunks[n]
        b = n % 5
        nj = 2 * i + 2
        ob = 5 + h // 4
        O3 = PS[ob][:, :].rearrange("p (r q) -> p r q", r=4)
        L = len(Js)
        m_ = Js[0] // 4
        partial = (Js[-1] != 4 * m_ + 3)
        assert (not partial) or (i % 2 == 0 and L == 2)
        bias_ap = ubl[:, i, h:h + 1] if partial else ub[:, i, m_, h:h + 1]
        scl_ap = kscB[:, i // 2, :, h] if partial else kscA[:, 4 * m_:4 * m_ + 4, h]
        pt_res = [r_PT[b]] + r_PTs[b][0:L]
        act(PT[b][:, 0:L * 128], PS[b][:, 0:L * 128], AF.Exp, reads=[PSR[b], r_fl], writes=pt_res, bias=bias_ap)
        S.op("dve", "tensor_tensor", reads=[r_fl] + pt_res, writes=pt_res,
             out=PT[b][:, 0:L * 128].rearrange("p (s q) -> p s q", s=L),
             in0=PT[b][:, 0:L * 128].rearrange("p (s q) -> p s q", s=L), in1=bc_last(scl_ap, 128), op=ALU.mult)
        for s_, J in enumerate(Js):
            if J >= 2 * i:
                S.op("dve", "tensor_tensor", reads=[r_PTs[b][s_], r_tbl], writes=[r_PTs[b][s_]],
                     out=PT[b][:, s_ * 128:(s_ + 1) * 128], in0=PT[b][:, s_ * 128:(s_ + 1) * 128],
                     in1=mpair[:, i, J - 2 * i, :], op=ALU.mult)
        for s_, J in enumerate(Js):
            mm(O3[:, h % 4, 0:65], PT[b][:, s_ * 128:(s_ + 1) * 128], Vf[:, J, h, :], J == 0, J == nj - 1,
               reads=[r_PT[b], r_PTs[b][s_], r_Vf[J]], writes=[PSR[ob]], last=(s_ == len(Js) - 1))
        if Js[-1] == nj - 1 and h % 4 == 3:
            k = fin_i[0] % 2
            fin_i[0] += 1
            S.op("dve", "reciprocal", reads=[PSR[ob]], writes=[r_rden[k]], out=rden[:, k, 0:4], in_=O3[:, :, 64])
            for r in range(4):
                hh = (h // 4) * 4 + r
                S.op("dve", "tensor_scalar", reads=[PSR[ob], r_rden[k]], writes=[r_fox_out[i]],
                     out=fox_out[:, i, hh * 64:(hh + 1) * 64], in0=O3[:, r, 0:64], scalar1=rden[:, k, r:r + 1],
                     scalar2=None, op0=ALU.mult)

    DEPTH = 4
    for n in range(min(DEPTH, len(chunks))):
        fox_S(n)
    for n in range(len(chunks)):
        if n + DEPTH < len(chunks):
            fox_S(n + DEPTH)
        fox_PV(n)

    if debug:
        S.dma("pool", reads=r_swa_out, out=dbg_swa.rearrange("(i p) f -> p i f", p=128), in_=swa_out[:])
        S.dma("pool", reads=r_fox_out, out=dbg_fox.rearrange("(i p) f -> p i f", p=128), in_=fox_out[:])
    S.barrier()
    A.release(m_attn)
    h1 = A.alloc([128, NOWN, 1024], F32)
    r_h1 = [Res() for _ in range(NOWN)]
    G = A.alloc([128, NOWN, 32], F32)
    gk = A.alloc([128, NOWN, 4], F32)
    slots_i = A.alloc([128, NOWN, 4], mybir.dt.int32)
    r_route = [Res() for _ in range(NOWN)]
    m_h1 = A.mark()
    lgall = A.alloc([128, NOWN, 32], F32)
    r_lg = [Res() for _ in range(NOWN)]
    m_r = A.mark()
    wr_sb = A.alloc([128, 8, 32], F32)
    br_sb = A.alloc([128, 32], F32)
    r_w4r = Res("w4r")
    S.dma("sp", writes=[r_w4r], out=wr_sb[:], in_=wr)
    S.dma("sp", writes=[r_w4r], out=br_sb[:], in_=br)
    hTf = A.alloc([128, 8, 128], F32)
    r_hTf = Res()
    wpa_sb = A.alloc([128, 4, 1024], BF16)
    wpf_sb = A.alloc([128, 4, 1024], BF16)
    wout_sb = A.alloc([128, 8, 1024], BF16)
    r_w3 = Res("w3")
    S.dma("pool", writes=[r_w3], out=wpa_sb[:], in_=wpa)
    S.dma("pool", writes=[r_w3], out=wpf_sb[:], in_=wpf)
    S.dma("pool", writes=[r_w3], out=wout_sb[:], in_=wout)
    g1 = A.alloc([128, 1024], F32)
    b1 = A.alloc([128, 1024], F32)
    S.dma("sp", writes=[r_w3], out=g1[:], in_=ln1g)
    S.dma("sp", writes=[r_w3], out=b1[:], in_=ln1b)
    xq = [A.alloc([128, 8, 512], BF16) for _ in range(1)]
    r_xq = [Res()]
    NWR = 6
    wring = [A.alloc([128, 8, 128], BF16) for _ in range(NWR)]
    r_wring = [Res() for _ in range(NWR)]
    aT = A.alloc([128, 4, 512], BF16)
    fT = A.alloc([128, 4, 512], BF16)
    r_aT, r_fT = Res(), Res()
    mixT = [A.alloc([128, 8, 512], BF16) for _ in range(2)]
    r_mixT = [[Res() for _ in range(8)] for _ in range(2)]
    sg = [A.alloc([128, 512], F32) for _ in range(4)]
    r_sg = [Res() for _ in range(4)]
    xres = [A.alloc([128, 1024], F32) for _ in range(1)]
    r_xres = [Res()]
    lnt = A.alloc([128, 1024], BF16)
    r_lnt = Res()
    stat = A.alloc([128, 4, 8], F32)
    r_stat = [Res() for _ in range(4)]

    ln_i = [0]

    def ln_stages(src, r_src, dst, r_dst, gam, bet, r_gb):
        k_ = ln_i[0] % 4
        ln_i[0] += 1
        st_ = stat[:, k_, :]
        rs = r_stat[k_]

        def s0():
            S.op("dve", "memset", writes=[rs], ap=st_[:, 0:4], constant=0.0)
            act(lnt[:], src, AF.Identity, reads=[r_src, rs], writes=[rs], accum_out=st_[:, 0:1])

        def s1():
            S.op("dve", "tensor_scalar", reads=[rs], writes=[rs], out=st_[:, 1:2], in0=st_[:, 0:1],
                 scalar1=-1.0 / 1024, scalar2=None, op0=ALU.mult)
            act(lnt[:], src, AF.Square, reads=[r_src, rs], writes=[rs], bias=st_[:, 1:2], accum_out=st_[:, 2:3])

        def s2():
            S.op("dve", "tensor_scalar", reads=[rs], writes=[rs], out=st_[:, 3:4], in0=st_[:, 2:3],
                 scalar1=1.0 / 1024, scalar2=EPS, op0=ALU.mult, op1=ALU.add)
            act(st_[:, 5:6], st_[:, 3:4], AF.Sqrt, reads=[rs], writes=[rs])

        def s3():
            S.op("dve", "reciprocal", reads=[rs], writes=[rs], out=st_[:, 4:5], in_=st_[:, 5:6])
            S.op("dve", "tensor_tensor", reads=[rs], writes=[rs], out=st_[:, 6:7], in0=st_[:, 1:2], in1=st_[:, 4:5],
                 op=ALU.mult)
            act(dst, src, AF.Identity, reads=[r_src, rs], writes=[r_dst], scale=st_[:, 4:5], bias=st_[:, 6:7])

        def s4():
            S.op("dve", "tensor_tensor", reads=[r_dst, r_gb], writes=[r_dst], out=dst, in0=dst, in1=gam, op=ALU.mult)
            S.op("dve", "tensor_tensor", reads=[r_dst, r_gb], writes=[r_dst], out=dst, in0=dst, in1=bet, op=ALU.add)
        return [s0, s1, s2, s3, s4]

    def layer_norm(src, r_src, dst, r_dst, gam, bet, r_gb):
        for f_ in ln_stages(src, r_src, dst, r_dst, gam, bet, r_gb):
            f_()

    def route_pe(i):
        for half in range(2):
            b = nbank(0, 7)
            for cc in range(4):
                c = half * 4 + cc
                S.op("pe", "transpose", reads=[r_h1[i], r_const], writes=[PSR[b]], inc=(cc == 3),
                     out=PS[b][:, cc * 128:(cc + 1) * 128], in_=h1[:, i, c * 128:(c + 1) * 128], identity=ident_f[:])
            S.op("dve", "tensor_copy", reads=[PSR[b]], writes=[r_hTf],
                 out=hTf[:, half * 4:(half + 1) * 4, :], in_=PS[b][:, :].rearrange("p (c t) -> p c t", c=4))
        b = nbank(0, 7)
        for c in range(8):
            mm(PS[b][:, 0:32], hTf[:, c, :], wr_sb[:, c, :], c == 0, c == 7, reads=[r_hTf, r_w4r], writes=[PSR[b]])
        S.op("dve", "tensor_tensor", reads=[PSR[b], r_w4r], writes=[r_lg[i]], out=lgall[:, i, :], in0=PS[b][:, 0:32],
             in1=br_sb[:], op=ALU.add)

    wr_i = [0]

    def gate_prep(og):
        S.dma("pool", writes=[r_xq[0]], out=xq[0][:], in_=xTo3[:, :, og * 512:(og + 1) * 512])
        for src, r_src, dstT, r_dstT in ((swa_out, r_swa_out, aT, r_aT), (fox_out, r_fox_out, fT, r_fT)):
            for half in range(2):
                for bb in range(2):
                    i = og * 4 + half * 2 + bb
                    for r in range(4):
                        S.op("pe", "transpose", reads=[r_src[i], r_const], writes=[PSTR],
                             inc=(bb == 1 and r == 3),
                             out=PST[:, (bb * 4 + r) * 128:(bb * 4 + r + 1) * 128],
                             in_=src[:, i, r * 128:(r + 1) * 128], identity=ident_b[:])
                S.op("dve", "tensor_copy", reads=[PSTR], writes=[r_dstT],
                     out=dstT[:, :, half * 256:(half + 1) * 256].rearrange("p r (b t) -> p b r t", b=2),
                     in_=PST[:, :].rearrange("p (b r t) -> p b r t", b=2, r=4))

    def gate_j(og, j):
        mb = og % 2
        bsel = []
        for which in range(2):
            ws = wr_i[0] % NWR
            wr_i[0] += 1
            S.dma("pool", writes=[r_wring[ws]], out=wring[ws][:].rearrange("p c f -> p (c f)"),
                  in_=wfm[13 + which * 8 + j])
            b = nbank(0, 7)
            for c in range(8):
                mm(PS[b][:, :], wring[ws][:, c, :], xq[0][:, c, :], c == 0, c == 7,
                   reads=[r_wring[ws], r_xq[0]], writes=[PSR[b]])
            act(sg[which][:], PS[b][:, :], AF.Sigmoid, reads=[PSR[b]], writes=[r_sg[which]])
        for which, (wsb, srcT, r_srcT) in enumerate(((wpa_sb, aT, r_aT), (wpf_sb, fT, r_fT))):
            b = nbank(0, 7)
            bsel.append(b)
            for r in range(4):
                mm(PS[b][:, :], wsb[:, r, j * 128:(j + 1) * 128], srcT[:, r, :], r == 0, r == 3,
                   reads=[r_w3, r_srcT], writes=[PSR[b]])
        S.op("dve", "tensor_tensor", reads=[PSR[bsel[0]], r_sg[0]], writes=[r_sg[2]], out=sg[2][:],
             in0=PS[bsel[0]][:, :], in1=sg[0][:], op=ALU.mult)
        S.op("dve", "tensor_tensor", reads=[PSR[bsel[1]], r_sg[1]], writes=[r_sg[3]], out=sg[3][:],
             in0=PS[bsel[1]][:, :], in1=sg[1][:], op=ALU.mult)
        S.op("dve", "tensor_tensor", reads=[r_sg[2], r_sg[3]], writes=[r_mixT[mb][j]], out=mixT[mb][:, j, :],
             in0=sg[2][:], in1=sg[3][:], op=ALU.add)

    def tile_z(og, blk):
        mb = og % 2
        i = og * 4 + blk
        xs_ = 0
        S.dma("sp", writes=[r_xres[xs_]], out=xres[xs_][:], in_=xo[i * 128:(i + 1) * 128, :])
        for half in range(2):
            b = nbank(0, 7)
            for c in range(8):
                mm(PS[b][:, :], mixT[mb][:, c, blk * 128:(blk + 1) * 128], wout_sb[:, c, half * 512:(half + 1) * 512],
                   c == 0, c == 7, reads=[r_mixT[mb][c], r_w3], writes=[PSR[b]])
            S.op("dve", "scalar_tensor_tensor", reads=[PSR[b], r_xres[xs_]], writes=[r_h1[i]],
                 out=h1[:, i, half * 512:(half + 1) * 512], in0=xres[xs_][:, half * 512:(half + 1) * 512],
                 scalar=ALPHA, in1=PS[b][:, :], op0=ALU.mult, op1=ALU.add)
        if i > 0:
            route_pe(i - 1)
        layer_norm(h1[:, i, :], r_h1[i], h1[:, i, :], r_h1[i], g1[:], b1[:], r_w3)
        if debug:
            S.dma("sp", reads=[r_h1[i]], out=dbg_h1[i * 128:(i + 1) * 128, :], in_=h1[:, i, :])

    gate_prep(0)
    for j in range(8):
        gate_j(0, j)
    for og in range(4):
        if og + 1 < 4:
            gate_prep(og + 1)
        for blk in range(4):
            if og + 1 < 4:
                gate_j(og + 1, 2 * blk)
                gate_j(og + 1, 2 * blk + 1)
            tile_z(og, blk)

    route_pe(NOWN - 1)
    S.barrier()
    A.release(m_r)
    A.top = A.limit
    wdb = [A.alloc([128, 8, 1024], BF16) for _ in range(3)]
    r_wdb = [[Res() for _ in range(8)] for _ in range(3)]
    NWG = 8
    wgr = [A.alloc([128, 8, 128], BF16) for _ in range(NWG)]
    r_wgr = [Res() for _ in range(NWG)]
    bgu_sb = A.alloc([128, NE * 16], F32)
    r_w4 = Res("w4")
    S.dma("sp", writes=[r_w4], out=bgu_sb[:], in_=bgu)
    m_w = A.mark()
    wg_issued = [0]
    wd_issued = [0]

    def issue_wg(upto):
        while wg_issued[0] < min(upto, NE * 16):
            n_ = wg_issued[0]
            e_, j_, wh_ = n_ // 16, (n_ % 16) // 2, n_ % 2
            ws_ = n_ % NWG
            S.dma("pool", writes=[r_wgr[ws_]], out=wgr[ws_][:].rearrange("p c f -> p (c f)"),
                  in_=(wgu[e_, j_ + 8 * wh_] if e_ < E_PRE0 else wgu_b[e_ - E_PRE0, j_ + 8 * wh_]))
            wg_issued[0] += 1

    def issue_wd(upto_e):
        while wd_issued[0] < min(upto_e, NE):
            e_ = wd_issued[0]
            for kk in range(8):
                S.dma("pool", writes=[r_wdb[e_ % 3][kk]], out=wdb[e_ % 3][:, kk, :],
                      in_=(wd[e_, kk] if e_ < E_PRE0 else wd_b[e_ - E_PRE0, kk]))
            wd_issued[0] += 1

    issue_wg(NWG)
    issue_wd(1)
    ebase_sb = A.alloc([128, 32], F32)
    striu_b = A.alloc([128, 128], BF16)
    ones_b = A.alloc([128, 128], BF16)
    S.dma("sp", writes=[r_w4r], out=ebase_sb[:], in_=ebase)
    S.dma("pool", writes=[r_w4r], out=striu_b[:], in_=striu_d)
    S.op("pool", "memset", writes=[r_w4r], ap=ones_b[:], constant=1.0)
    top8a = A.alloc([128, NOWN, 8], F32)
    mska = A.alloc([128, NOWN, 32], F32)
    mskb = A.alloc([128, NOWN, 32], BF16)
    exa = A.alloc([128, NOWN, 32], F32)
    rka = A.alloc([128, NOWN, 32], F32)
    sfa = A.alloc([128, NOWN, 32], F32)
    ova = A.alloc([128, NOWN, 32], F32)
    nva = A.alloc([128, NOWN, 32], F32)
    oha = A.alloc([128, NOWN, 32], F32)
    t32a = A.alloc([128, NOWN, 32], F32)
    sma = A.alloc([128, NOWN], F32)
    rca = A.alloc([128, NOWN], F32)
    slots_f = A.alloc([128, NOWN, 4], F32)
    h1b = [A.alloc([128, 1024], BF16) for _ in range(2)]
    r_h1b = [Res(), Res()]
    r_rt = Res("router")

    def dv(name, reads=(), writes=(), **kw):
        S.op("dve", name, reads=[r_rt] + list(reads), writes=[r_rt] + list(writes), **kw)
    for i in range(NOWN):
        dv("max", reads=[r_lg[i]], out=top8a[:, i, :], in_=lgall[:, i, :])
    dv("tensor_tensor", out=mska[:], in0=lgall[:], in1=bc_last(top8a[:, :, 3], 32), op=ALU.is_ge)
    dv("tensor_copy", out=mskb[:], in_=mska[:])
    dv("tensor_tensor", out=exa[:], in0=lgall[:], in1=bc_last(top8a[:, :, 0], 32), op=ALU.subtract)
    act(exa[:], exa[:], AF.Exp, reads=[r_rt], writes=[r_rt])
    dv("tensor_tensor", out=exa[:], in0=exa[:], in1=mska[:], op=ALU.mult)
    dv("tensor_reduce", out=sma[:], in_=exa[:], axis=AX.X, op=ALU.add)
    dv("reciprocal", out=rca[:], in_=sma[:])
    rb = nbank(0, 7)
    for i in range(NOWN):
        for i2 in range(i):
            mm(PS[rb][:, i * 32:(i + 1) * 32], ones_b[:], mskb[:, i2, :], i2 == 0, False, reads=[r_rt, r_w4r],
               writes=[PSR[rb]], last=False)
        mm(PS[rb][:, i * 32:(i + 1) * 32], striu_b[:], mskb[:, i, :], i == 0, True, reads=[r_rt, r_w4r],
           writes=[PSR[rb]])
    dv("tensor_copy", reads=[PSR[rb]], out=rka[:].rearrange("p a b -> p (a b)"), in_=PS[rb][:, :])
    eb_ap = ebase_sb[:, :]
    ebase_bc = bass.AP(eb_ap.tensor, eb_ap.offset, [list(eb_ap.ap[0]), [0, NOWN], list(eb_ap.ap[1])])
    dv("tensor_scalar", out=ova[:], in0=rka[:], scalar1=float(CAP), scalar2=None, op0=ALU.is_ge)
    dv("tensor_tensor", reads=[r_w4r], out=sfa[:], in0=rka[:], in1=ebase_bc, op=ALU.add)
    dv("tensor_scalar", out=nva[:], in0=ova[:], scalar1=-1.0, scalar2=1.0, op0=ALU.mult, op1=ALU.add)
    dv("tensor_tensor", out=sfa[:], in0=sfa[:], in1=nva[:], op=ALU.mult)
    dv("scalar_tensor_tensor", out=sfa[:], in0=ova[:], scalar=float(NSLOT), in1=sfa[:], op0=ALU.mult, op1=ALU.add)
    dv("tensor_tensor", out=exa[:], in0=exa[:], in1=bc_last(rca[:, :], 32), op=ALU.mult)
    dv("tensor_tensor", writes=r_route, out=G[:], in0=exa[:], in1=nva[:], op=ALU.mult)
    for k in range(4):
        dv("tensor_tensor", out=oha[:], in0=lgall[:], in1=bc_last(top8a[:, :, k], 32), op=ALU.is_equal)
        dv("tensor_tensor", out=t32a[:], in0=oha[:], in1=sfa[:], op=ALU.mult)
        dv("tensor_reduce", out=slots_f[:, :, k], in_=t32a[:], axis=AX.X, op=ALU.add)
        dv("tensor_tensor", out=t32a[:], in0=oha[:], in1=G[:], op=ALU.mult)
        dv("tensor_reduce", writes=r_route, out=gk[:, :, k], in_=t32a[:], axis=AX.X, op=ALU.add)
    dv("tensor_copy", writes=r_route, out=slots_i[:], in_=slots_f[:])
    for i in range(NOWN):
        k2 = i % 2
        act(h1b[k2][:], h1[:, i, :], AF.Copy, reads=[r_h1[i]], writes=[r_h1b[k2]])
        for k in range(4):
            S.dma("pool", name="indirect_dma_start", reads=[r_h1b[k2], r_route[i]], writes=[],
                  out=xs_d[:, :], out_offset=bass.IndirectOffsetOnAxis(ap=slots_i[:, i, k:k + 1], axis=0),
                  in_=h1b[k2][:, :], in_offset=None, bounds_check=None, oob_is_err=False)
        if debug:
            S.dma("sp", reads=[r_route[i]], out=dbg_G[i * 128:(i + 1) * 128, :], in_=G[:, i, :])
        act(h1[:, i, :], h1[:, i, :], AF.Copy, reads=[r_h1[i]], writes=[r_h1[i]], scale=ALPHA)

    S.barrier()
    A.release(m_w)
    A.top = A.limit
    xrows = [A.alloc([128, CAP // 128, 1024], BF16) for _ in range(2)]
    r_xrows = [Res(), Res()]
    xsT = [A.alloc([128, 8, CAP], BF16) for _ in range(2)]
    r_xsT = [Res(), Res()]
    actT = [A.alloc([128, 8, CAP], BF16) for _ in range(2)]
    r_actT = [[Res() for _ in range(8)] for _ in range(2)]
    ystage = [A.alloc([128, 1024], F32) for _ in range(2)]
    r_ystage = [Res(), Res()]
    bdb = [A.alloc([128, 1024], F32) for _ in range(2)]
    r_bdb = [Res(), Res()]
    tg_ = [A.alloc([128, CAP], F32) for _ in range(2)]
    ts_ = [A.alloc([128, CAP], F32) for _ in range(2)]
    tu_ = [A.alloc([128, CAP], F32) for _ in range(2)]
    r_tg = [Res(), Res()]
    r_ts = [Res(), Res()]
    r_tu = [Res(), Res()]
    NST = CAP // 128
    wg_i = [0]
    tmp_i = [0]
    ys_i = [0]
    def load_xrows(e_):
        S.dma("sp", writes=[r_xrows[e_ % 2]], out=xrows[e_ % 2][:],
              in_=xs_d[e_ * CAP:(e_ + 1) * CAP, :].rearrange("(s p) f -> p s f", p=128))
    load_xrows(0)

    def exp_front(e):
        wb = e % 2
        if e + 1 < NE:
            load_xrows(e + 1)
        bd_row = bd[e:e + 1, :]
        S.dma("sp", writes=[r_bdb[wb]], out=bdb[wb][:],
              in_=bass.AP(bd_row.tensor, bd_row.offset, [[0, 128], [1, 1024]]))
        for st in range(NST):
            tb_, tr_ = (PST[:, :], PSTR) if st % 2 == 0 else (PS[6][:, :].bitcast(BF16), PSR[6])
            for c in range(8):
                S.op("pe", "transpose", reads=[r_xrows[wb], r_const], writes=[tr_], inc=(c == 7),
                     out=tb_[:, c * 128:(c + 1) * 128], in_=xrows[wb][:, st, c * 128:(c + 1) * 128],
                     identity=ident_b[:])
            act(xsT[wb][:, :, st * 128:(st + 1) * 128], tb_.rearrange("p (c t) -> p c t", c=8), AF.Copy,
                reads=[tr_], writes=[r_xsT[wb]])
        for j in range(8):
            n0 = e * 16 + j * 2
            issue_wg(n0 + 2)
            slots = [n0 % NWG, (n0 + 1) % NWG]
            bg = nbank(0, 6)
            for c in range(8):
                mm(PS[bg][:, 0:CAP], wgr[slots[0]][:, c, :], xsT[wb][:, c, :], c == 0, c == 7,
                   reads=[r_wgr[slots[0]], r_xsT[wb]], writes=[PSR[bg]])
            bu = nbank(0, 6)
            for c in range(8):
                mm(PS[bu][:, 0:CAP], wgr[slots[1]][:, c, :], xsT[wb][:, c, :], c == 0, c == 7,
                   reads=[r_wgr[slots[1]], r_xsT[wb]], writes=[PSR[bu]])
            k = tmp_i[0] % 2
            tmp_i[0] += 1
            colg = e * 16 + j
            colu = e * 16 + 8 + j
            S.op("dve", "tensor_scalar", reads=[PSR[bg], r_w4], writes=[r_tg[k]], out=tg_[k][:], in0=PS[bg][:, 0:CAP],
                 scalar1=bgu_sb[:, colg:colg + 1], scalar2=7.0, op0=ALU.add, op1=ALU.min)
            act(ts_[k][:], tg_[k][:], AF.Sigmoid, reads=[r_tg[k]], writes=[r_ts[k]], scale=1.702)
            act(tu_[k][:], PS[bu][:, 0:CAP], AF.Identity, reads=[PSR[bu], r_w4], writes=[r_tu[k]],
                bias=bgu_sb[:, colu:colu + 1])
            S.op("dve", "tensor_scalar", reads=[r_tu[k]], writes=[r_tu[k]], out=tu_[k][:], in0=tu_[k][:],
                 scalar1=7.0, scalar2=-7.0, op0=ALU.min, op1=ALU.max)
            S.op("dve", "scalar_tensor_tensor", reads=[r_tg[k], r_tu[k]], writes=[r_tu[k]], out=tu_[k][:], in0=tu_[k][:],
                 scalar=1.0, in1=tg_[k][:], op0=ALU.add, op1=ALU.mult)
            S.op("dve", "tensor_tensor", reads=[r_ts[k], r_tu[k]], writes=[r_actT[wb][j]],
                 out=actT[wb][:, j, :], in0=ts_[k][:], in1=tu_[k][:], op=ALU.mult)
        issue_wd(e + 1)

    def exp_back(e):
        wb = e % 2
        for st in range(NST):
            ysb = ys_i[0] % 2
            ys_i[0] += 1
            for half in range(2):
                b = nbank(0, 6)
                for kk in range(8):
                    mm(PS[b][:, :], actT[wb][:, kk, st * 128:(st + 1) * 128], wdb[e % 3][:, kk, half * 512:(half + 1) * 512],
                       kk == 0, kk == 7, reads=[r_actT[wb][kk], r_wdb[e % 3][kk]], writes=[PSR[b]])
                S.op("dve", "tensor_tensor", reads=[PSR[b], r_bdb[wb]], writes=[r_ystage[ysb]],
                     out=ystage[ysb][:, half * 512:(half + 1) * 512], in0=PS[b][:, :],
                     in1=bdb[wb][:, half * 512:(half + 1) * 512], op=ALU.add)
            r0 = e * CAP + st * 128
            S.dma("sp", reads=[r_ystage[ysb]], out=ys_d[r0:r0 + 128, :], in_=ystage[ysb][:])


    exp_front(0)
    for e in range(NE):
        if e + 1 < NE:
            exp_front(e + 1)
        exp_back(e)

    S.barrier()
    A.release(m_h1)
    g2 = A.alloc([128, 1024], F32)
    b2 = A.alloc([128, 1024], F32)
    S.dma("sp", writes=[r_w4], out=g2[:], in_=ln2g)
    S.dma("sp", writes=[r_w4], out=b2[:], in_=ln2b)
    lnt = A.alloc([128, 1024], F32)
    stat = A.alloc([128, 4, 8], F32)
    NYK = 5
    yk = [[A.alloc([128, 1024], F32) for _ in range(4)] for _ in range(NYK)]
    r_yk = [[Res() for _ in range(4)] for _ in range(NYK)]
    for a_ in range(NYK):
        for k in range(4):
            S.op("dve", "memset", writes=[r_yk[a_][k]], ap=yk[a_][k][:], constant=0.0)
    def gathers(i):
        a_ = i % NYK
        for k in range(4):
            S.dma("pool", name="indirect_dma_start", reads=[r_route[i]], writes=[r_yk[a_][k]],
                  out=yk[a_][k][:, :], out_offset=None, in_=ys_d[:, :],
                  in_offset=bass.IndirectOffsetOnAxis(ap=slots_i[:, i, k:k + 1], axis=0),
                  bounds_check=None, oob_is_err=False)

    def stt(i, k):
        a_ = i % NYK
        S.op("dve", "scalar_tensor_tensor", reads=[r_yk[a_][k], r_route[i], r_h1[i]], writes=[r_h1[i]],
             out=h1[:, i, :], in0=yk[a_][k][:], scalar=gk[:, i, k:k + 1], in1=h1[:, i, :],
             op0=ALU.mult, op1=ALU.add)

    for i in range(min(NYK - 1, NOWN)):
        gathers(i)
    for i in range(NOWN + 1):
        if i + NYK - 1 < NOWN:
            gathers(i + NYK - 1)
        st_prev = None
        if i >= 1:
            st_prev = ln_stages(h1[:, i - 1, :], r_h1[i - 1], h1[:, i - 1, :], r_h1[i - 1], g2[:], b2[:], r_w4)
            st_prev[0]()
        for k in range(4):
            if i < NOWN:
                stt(i, k)
            if st_prev is not None:
                st_prev[k + 1]()
        if i >= 1:
            S.dma("sp", reads=[r_h1[i - 1]], out=out[(i - 1) * 128:i * 128, :], in_=h1[:, i - 1, :])
    S.barrier()
    S.emit()
    return nc


def _prep(inp):
    f = np.float32
    x = np.asarray(inp["x"], f)
    w_in = np.asarray(inp["w_in"], f)[0]
    sh = {}

    def fm(cols):
        return np.ascontiguousarray(w_in[:, cols].reshape(8, 128, 128).transpose(1, 0, 2).reshape(128, 1024))
    pieces = []
    for j in range(4):
        pieces.append(fm(np.arange(C_FOXK + 128 * j, C_FOXK + 128 * (j + 1))))
    pieces.append(fm(np.arange(C_SWAK, C_SWAK + 128)))
    for j in range(4):
        pieces.append(fm(np.arange(C_FOXQ + 128 * j, C_FOXQ + 128 * (j + 1))))
    for r in range(4):
        cols = np.concatenate([np.arange(C_SWAQ + r * 64, C_SWAQ + (r + 1) * 64),
                               np.arange(C_SWAQ + (4 + r) * 64, C_SWAQ + (5 + r) * 64)])
        pieces.append(fm(cols))
    for j in range(8):
        pieces.append(fm(np.arange(C_GA + 128 * j, C_GA + 128 * (j + 1))))
    for j in range(8):
        pieces.append(fm(np.arange(C_GF + 128 * j, C_GF + 128 * (j + 1))))
    sh["wfm"] = np.stack(pieces)
    cols = np.concatenate([np.arange(C_FOXV, C_FOXV + 512), np.arange(C_FL, C_FL + 8), np.arange(C_SWAV, C_SWAV + 128)])
    sh["wtm"] = np.ascontiguousarray(w_in[:, cols].reshape(8, 128, 648).transpose(1, 0, 2))
    sh["wpa"] = np.ascontiguousarray(np.asarray(inp["w_proj_swa"], f)[0].reshape(4, 128, 1024).transpose(1, 0, 2))
    sh["wpf"] = np.ascontiguousarray(np.asarray(inp["w_proj_fox"], f)[0].reshape(4, 128, 1024).transpose(1, 0, 2))
    sh["wout"] = np.ascontiguousarray(np.asarray(inp["w_out"], f)[0].reshape(8, 128, 1024).transpose(1, 0, 2))
    rep = lambda v, n=128: np.ascontiguousarray(np.broadcast_to(np.asarray(v, f).reshape(1, -1), (n, np.asarray(v).size)))
    sh["ln1g"] = rep(inp["ln1_g"][0])
    sh["ln1b"] = rep(inp["ln1_b"][0])
    sh["ln2g"] = rep(inp["ln2_g"][0])
    sh["ln2b"] = rep(inp["ln2_b"][0])
    sh["wr"] = np.ascontiguousarray(np.asarray(inp["w_router"], f)[0].reshape(8, 128, 32).transpose(1, 0, 2))
    sh["br"] = rep(inp["b_router"][0])
    sh["bfr"] = rep(np.tile(np.asarray(inp["b_forget"], f)[0], NB))
    sh["sink8"] = rep(np.asarray(inp["sink"], f)[0].reshape(-1))
    sh["ident"] = np.eye(128, dtype=f)
    sh["triu"] = np.triu(np.ones((128, 128), f))
    wguf = np.asarray(inp["w_gate_up"], f)[0]
    sh["wgu"] = np.ascontiguousarray(wguf.reshape(NE, 8, 128, 16, 128).transpose(0, 3, 2, 1, 4).reshape(NE, 16, 128, 1024))
    sh["wd"] = np.ascontiguousarray(np.asarray(inp["w_down"], f)[0].reshape(NE, 8, 128, 1024))
    bguf = np.asarray(inp["b_gate_up"], f)[0]
    sh["bgu"] = np.ascontiguousarray(bguf.reshape(NE, 16, 128).transpose(2, 0, 1).reshape(128, NE * 16))
    sh["bd"] = np.ascontiguousarray(np.asarray(inp["b_down"], f)[0])
    sh["ebase"] = np.ascontiguousarray(np.broadcast_to((np.arange(NE, dtype=f) * CAP).reshape(1, NE), (128, NE)))
    sh["striu"] = np.triu(np.ones((128, 128), f), 1)

    slopes = (2.0 ** (-8.0 * np.arange(1, 9) / 8)).astype(np.float64)
    kk = np.arange(128)[:, None]
    qq = np.arange(128)[None, :]
    tb = np.full((128, 3, 2, 4, 128), NEG, np.float64)
    for g in range(2):
        for r in range(4):
            sl = slopes[4 * g + r]
            dist = qq - kk
            tb[:, 0, g, r, :] = np.where(dist >= 0, -sl * dist, NEG)
            dist = qq + 128 - kk
            tb[:, 1, g, r, :] = np.where(dist < 128, -sl * dist, NEG)
    tri = (kk <= qq).astype(f)

    percore = []
    own_all = []
    for c in range(8):
        b, p = c // 2, c % 2
        own = [I for I in range(NB) if ((I % 4 in (0, 3)) == (p == 0))]
        own_all.append(own)
        xb = x[b]
        xT = xb.T
        d = {}
        d["xT3"] = np.ascontiguousarray(xT.reshape(8, 128, 4096).transpose(1, 0, 2))
        tok_own = np.concatenate([np.arange(I * 128, (I + 1) * 128) for I in own])
        d["xTo3"] = np.ascontiguousarray(xT[:, tok_own].reshape(8, 128, 2048).transpose(1, 0, 2))
        d["xo"] = np.ascontiguousarray(xb[tok_own])
        xw = np.zeros((1024, 4096), f)
        for i, I in enumerate(own):
            if I > 0:
                xw[:, (2 * i) * 128:(2 * i + 1) * 128] = xT[:, (I - 1) * 128:I * 128]
            xw[:, (2 * i + 1) * 128:(2 * i + 2) * 128] = xT[:, I * 128:(I + 1) * 128]
        d["xTw3"] = np.ascontiguousarray(xw.reshape(8, 128, 4096).transpose(1, 0, 2))
        t = tb.copy()
        t[:, 2] = t[:, 1] if own[0] > 0 else NEG
        d["swatbl"] = np.ascontiguousarray(t.reshape(128, -1).astype(f))
        mp = np.zeros((128, 16, 2, 128), f)
        pr = np.zeros((128, 16), f)
        for i, I in enumerate(own):
            if I == 2 * i:
                mp[:, i, 0, :] = tri
                mp[:, i, 1, :] = 0.0
            else:
                assert I == 2 * i + 1
                mp[:, i, 0, :] = 1.0
                mp[:, i, 1, :] = tri
                pr[:, i] = 1.0
        d["maskpair"] = np.ascontiguousarray(mp.reshape(128, -1))
        d["par"] = pr
        d.update(sh)
        percore.append(d)
    return percore, own_all


_NC_CACHE = {}


def kernel(**inputs):
    percore, own_all = _prep(inputs)
    if "nc" not in _NC_CACHE:
        _NC_CACHE["nc"] = build_nc()
    nc = _NC_CACHE["nc"]
    res = run_bass_kernel_spmd(nc, percore, core_ids=list(range(8)))
    outp = np.zeros((4, 4096, 1024), np.float32)
    for c in range(8):
        o = np.asarray(res.results[c]["out"], np.float32)
        for i, I in enumerate(own_all[c]):
            outp[c // 2, I * 128:(I + 1) * 128, :] = o[i * 128:(i + 1) * 128]
    return outp
```
